# Optimizing a Trainium2 kernel written in Bass

```python
import jax
import jax.numpy as jnp
from jax import lax
import numpy as np

D_MODEL = 1024
BATCH = 2
SEQ = 16384
DEPTH = 2

GRID_W = 64
CTX_LEN = 256

HEAD_DIM = 64
NA_WIDTH = D_MODEL // 2
NA_HEADS = NA_WIDTH // HEAD_DIM
NA_DH = HEAD_DIM
NA_KH = 8
NA_KW = 16
RW_WIDTH = D_MODEL // 4
RW_HEADS = RW_WIDTH // HEAD_DIM
RW_DH = HEAD_DIM
RW_DECAY_LORA = 32
RW_AAA_LORA = 32
RW_GATE_LORA = 64
RW_GN_EPS = 64e-5
RW_PROJ = 3 * RW_WIDTH + 2 * RW_DECAY_LORA + 2 * RW_AAA_LORA + RW_GATE_LORA
ML_WIDTH = D_MODEL // 4
ML_HEADS = ML_WIDTH // HEAD_DIM
ML_DQK = HEAD_DIM
ML_DV = HEAD_DIM
ML_QK_WIDTH = ML_HEADS * ML_DQK
ML_CONV = 3
ML_CHUNK = 64
ML_NORM_EPS = 1e-6
ML_PROJ = 2 * ML_QK_WIDTH + 2 * ML_WIDTH + 4 * ML_HEADS

MIX_WIDTH = NA_WIDTH + RW_WIDTH + ML_WIDTH
IN_PROJ = 3 * NA_WIDTH + RW_PROJ + ML_PROJ
ROPE_BASE = 10000.0
LN_EPS = 1e-5
N_GROUPS = 4
EXPERTS_PER_GROUP = 8
N_EXPERTS = N_GROUPS * EXPERTS_PER_GROUP
TOP_K = 2
D_EXPERT = D_MODEL // 2
MOE_BLOCK = 256

kernel_name = 'hybrid_natten_rwkv7_mlstm_hmoe'


def layer_norm(x, g, b):
    xf = x.astype(jnp.float32)
    mu = jnp.mean(xf, -1, keepdims=True)
    var = jnp.mean(jnp.square(xf - mu), -1, keepdims=True)
    return ((xf - mu) * lax.rsqrt(var + LN_EPS)).astype(x.dtype) * g + b


def head_norm(y, eps):
    yf = y.astype(jnp.float32)
    mu = jnp.mean(yf, -1, keepdims=True)
    var = jnp.mean(jnp.square(yf - mu), -1, keepdims=True)
    yn = (yf - mu) * lax.rsqrt(var + eps)
    return yn.reshape(*y.shape[:-2], -1)


def split_heads(t, n_heads):
    return t.reshape(*t.shape[:-1], n_heads, t.shape[-1] // n_heads)


def centred_shift_mix(p, mu_prev, mu_next):
    z = jnp.zeros_like(p[:, :1])
    prev = jnp.concatenate([z, p[:, :-1]], axis=1)
    nxt = jnp.concatenate([p[:, 1:], z], axis=1)
    return p + mu_prev * (prev - p) + mu_next * (nxt - p)


def centred_dwconv(p, w, b):
    pad = w.shape[0] // 2
    out = lax.conv_general_dilated(p, w[:, None, :].astype(p.dtype), window_strides=(1,), padding=[(pad, pad)],
                                   dimension_numbers=('NWC', 'WIO', 'NWC'), feature_group_count=p.shape[-1])
    return out + b


def axial_rope(t):
    n_tok, d = t.shape[1], t.shape[-1]
    half = d // 2
    nf = half // 2
    inv_freq = ROPE_BASE ** (-jnp.arange(nf, dtype=jnp.float32) / nf)
    pos = jnp.arange(n_tok, dtype=jnp.int32)

    def rotate(u, p):
        ang = p.astype(jnp.float32)[:, None] * inv_freq[None, :]
        cos = jnp.cos(ang)[None, :, None, :].astype(u.dtype)
        sin = jnp.sin(ang)[None, :, None, :].astype(u.dtype)
        u1, u2 = u[..., :nf], u[..., nf:]
        return jnp.concatenate([u1 * cos - u2 * sin, u1 * sin + u2 * cos], axis=-1)

    return jnp.concatenate([rotate(t[..., :half], pos // GRID_W), rotate(t[..., half:], pos % GRID_W)], axis=-1)


def neighbourhood_attention(q, k, v, k_ctx, v_ctx, rpb):
    B, n_tok, H, dh = q.shape
    rows = n_tok // GRID_W
    kh, kw = min(NA_KH, rows), NA_KW
    scale = dh ** -0.5
    qg = q.reshape(B, rows, GRID_W, H, dh)
    kg = k.reshape(B, rows, GRID_W, H, dh)
    vg = v.reshape(B, rows, GRID_W, H, dh)
    col = np.arange(GRID_W, dtype=np.int32)
    col_start = np.clip(col - kw // 2, 0, GRID_W - kw).astype(np.int32)
    col_idx = (col_start[:, None] + np.arange(kw, dtype=np.int32)[None, :]).astype(np.int32)
    bias_c = rpb[:, :, (col_idx - col[:, None] + (NA_KW - 1)).astype(np.int32)]

    def one_row(r):
        rs = jnp.clip(r - kh // 2, 0, rows - kh)
        q_r = lax.dynamic_index_in_dim(qg, r, axis=1, keepdims=False)
        k_w = lax.dynamic_slice_in_dim(kg, rs, kh, axis=1)[:, :, col_idx]
        v_w = lax.dynamic_slice_in_dim(vg, rs, kh, axis=1)[:, :, col_idx]
        bias = jnp.take(bias_c, rs + jnp.arange(kh, dtype=jnp.int32) - r + (NA_KH - 1), axis=1)
        s_loc = (jnp.einsum('bqhd,brqchd->bhqrc', q_r, k_w).astype(jnp.float32) * scale
                 + jnp.transpose(bias, (0, 2, 1, 3)).astype(jnp.float32)[None])
        s_ctx = jnp.einsum('bqhd,bkhd->bhqk', q_r, k_ctx).astype(jnp.float32) * scale
        s = jnp.concatenate([s_loc.reshape(B, H, GRID_W, kh * kw), s_ctx], axis=-1)
        p = jax.nn.softmax(s, axis=-1).astype(v.dtype)
        p_loc = p[..., :kh * kw].reshape(B, H, GRID_W, kh, kw)
        return (jnp.einsum('bhqrc,brqchd->bqhd', p_loc, v_w)
                + jnp.einsum('bhqk,bkhd->bqhd', p[..., kh * kw:], v_ctx))

    out = lax.map(one_row, jnp.arange(rows, dtype=jnp.int32))
    return jnp.moveaxis(out, 0, 1).reshape(B, n_tok, H * dh)


def context_attention(q, k, v):
    s = jnp.einsum('bqhd,bkhd->bhqk', q, k).astype(jnp.float32) * (q.shape[-1] ** -0.5)
    p = jax.nn.softmax(s, axis=-1).astype(v.dtype)
    out = jnp.einsum('bhqk,bkhd->bqhd', p, v)
    return out.reshape(*out.shape[:2], -1)


def na_mixer(pa_c, pa_l, rpb, need_ctx):
    q_c, k_c, v_c = [split_heads(t, NA_HEADS) for t in jnp.split(pa_c, 3, axis=-1)]
    q_l, k_l, v_l = [split_heads(t, NA_HEADS) for t in jnp.split(pa_l, 3, axis=-1)]
    y_l = neighbourhood_attention(q_l, k_l, v_l, k_c, v_c, rpb)
    y_c = context_attention(q_c, k_c, v_c) if need_ctx else None
    return y_c, y_l


def rwkv7_scan(r, w, k, v, a, b, S0, emit):
    xs = tuple(jnp.moveaxis(t.astype(jnp.float32), 1, 0) for t in (r, w, k, v, a, b))

    def step(S, inp):
        r_t, w_t, k_t, v_t, a_t, b_t = inp
        sa = jnp.einsum('bhvk,bhk->bhv', S, a_t)
        S = S * w_t[:, :, None, :] + sa[..., None] * b_t[:, :, None, :] + v_t[..., None] * k_t[:, :, None, :]
        return S, (jnp.einsum('bhvk,bhk->bhv', S, r_t) if emit else None)

    S, ys = lax.scan(step, S0, xs)
    return (jnp.moveaxis(ys, 0, 1) if emit else None), S


def _rwkv_features(pb, w0, w2, a0, a2, k_k, k_a):
    B, L, _ = pb.shape
    cuts = np.cumsum([RW_WIDTH, RW_WIDTH, RW_WIDTH, 2 * RW_DECAY_LORA, 2 * RW_AAA_LORA]).tolist()
    r, k, v, wd, ad, gd = jnp.split(pb.astype(jnp.float32), cuts, axis=-1)
    wd = jnp.tanh(wd.reshape(B, L, 2, RW_DECAY_LORA))
    ad = ad.reshape(B, L, 2, RW_AAA_LORA)
    log_w = -jax.nn.softplus(-(w0 + jnp.einsum('blzr,zrc->blzc', wd, w2))) - 0.5
    decay = jnp.exp(-jnp.exp(log_w))
    a = jax.nn.sigmoid(a0 + jnp.einsum('blzr,zrc->blzc', ad, a2))
    kk = split_heads(k * k_k, RW_HEADS)
    kk = kk / jnp.maximum(jnp.sqrt(jnp.sum(jnp.square(kk), -1, keepdims=True)), 1e-12)
    k_dir = k[:, :, None] * (1.0 + (a - 1.0) * k_a)
    b_dir = kk.reshape(B, L, 1, RW_WIDTH) * a
    hz = lambda t: split_heads(t, RW_HEADS)
    return hz(r), hz(k), hz(v), kk, gd, hz(decay), hz(k_dir), hz(b_dir)


def _rwkv_direction(f, d, S0, emit):
    r, k, v, kk, gd, decay, k_dir, b_dir = f
    seq = (r, decay[:, :, d], k_dir[:, :, d], v, -kk, b_dir[:, :, d])
    if d == 1:
        seq = tuple(jnp.flip(t, 1) for t in seq)
    y, S = rwkv7_scan(*seq, S0, emit)
    if emit and d == 1:
        y = jnp.flip(y, 1)
    return y, S


def _rwkv_output(y, f, g2, r_k, ln_w, ln_b, dtype):
    r, k, v, kk, gd = f[:5]
    bonus = jnp.sum(r * k * split_heads(r_k, RW_HEADS), -1, keepdims=True) * v
    out = head_norm(y, RW_GN_EPS) * ln_w + ln_b + bonus.reshape(*bonus.shape[:2], RW_WIDTH)
    gate = jnp.dot(jax.nn.sigmoid(gd), g2)
    return (out * gate).astype(dtype)


def rwkv7_mixer(pb_c, pb_l, mu_prev, mu_next, w0, w2, a0, a2, g2, k_k, k_a, r_k, ln_w, ln_b, need_ctx):
    f_c = _rwkv_features(centred_shift_mix(pb_c, mu_prev, mu_next), w0, w2, a0, a2, k_k, k_a)
    f_l = _rwkv_features(centred_shift_mix(pb_l, mu_prev, mu_next), w0, w2, a0, a2, k_k, k_a)
    S0 = jnp.zeros((pb_c.shape[0], RW_HEADS, RW_DH, RW_DH), jnp.float32)
    ys_c, ys_l = [], []
    for d in range(2):
        y_cd, S_ctx = _rwkv_direction(f_c, d, S0, need_ctx)
        y_ld, _ = _rwkv_direction(f_l, d, S_ctx, True)
        ys_c.append(y_cd)
        ys_l.append(y_ld)
    y_l = _rwkv_output(ys_l[0] + ys_l[1], f_l, g2, r_k, ln_w, ln_b, pb_l.dtype)
    y_c = _rwkv_output(ys_c[0] + ys_c[1], f_c, g2, r_k, ln_w, ln_b, pb_c.dtype) if need_ctx else None
    return y_c, y_l


def mlstm_chunk_scan(q, k, v, log_f, log_i, state, emit):
    B, H, L, _ = q.shape
    nc = L // ML_CHUNK

    def chunks(t):
        return jnp.moveaxis(t.reshape(B, H, nc, ML_CHUNK, *t.shape[3:]), 2, 0)

    causal = jnp.tril(jnp.ones((ML_CHUNK, ML_CHUNK), dtype=bool))

    def step(carry, inp):
        C, n, m = carry
        qc, kc, vc, lf, li = inp
        b = jnp.cumsum(lf, axis=-1)
        m_inter = b + m[..., None]
        g_last = b[..., -1:] - b + li
        m_last = jnp.maximum(m_inter[..., -1], jnp.max(g_last, -1))
        ws = jnp.exp(g_last - m_last[..., None])
        carry_decay = jnp.exp(m_inter[..., -1] - m_last)
        C_new = carry_decay[..., None, None] * C + jnp.einsum('bhs,bhsd,bhsv->bhdv', ws, kc, vc)
        n_new = carry_decay[..., None] * n + jnp.einsum('bhs,bhsd->bhd', ws, kc)
        if not emit:
            return (C_new, n_new, m_last), None
        log_d = jnp.where(causal, b[..., :, None] - b[..., None, :] + li[..., None, :], -jnp.inf)
        m_t = jnp.maximum(m_inter, jnp.max(log_d, -1))
        dw = jnp.exp(log_d - m_t[..., None]) * jnp.einsum('bhtd,bhsd->bhts', qc, kc)
        inter = jnp.exp(m_inter - m_t)
        num = jnp.einsum('bhts,bhsv->bhtv', dw, vc) + inter[..., None] * jnp.einsum('bhtd,bhdv->bhtv', qc, C)
        den = jnp.sum(dw, -1) + inter * jnp.einsum('bhtd,bhd->bht', qc, n)
        h = num / jnp.maximum(jnp.abs(den), jnp.exp(-m_t))[..., None]
        return (C_new, n_new, m_last), h

    state, hs = lax.scan(step, state, tuple(chunks(t) for t in (q, k, v, log_f, log_i)))
    if emit:
        hs = jnp.moveaxis(hs, 0, 2).reshape(B, H, L, -1)
    return hs, state


def _mlstm_prep(pc, conv_w, conv_b, i_bias, f_bias, use_rope):
    B, L, _ = pc.shape
    qk, v, o, gates = jnp.split(pc, [2 * ML_QK_WIDTH, 2 * ML_QK_WIDTH + ML_WIDTH, 2 * ML_QK_WIDTH + 2 * ML_WIDTH], axis=-1)
    qk = jax.nn.silu(centred_dwconv(qk, conv_w, conv_b)).astype(jnp.float32)
    q, k = jnp.split(qk, 2, axis=-1)
    q, k = split_heads(q, ML_HEADS), split_heads(k, ML_HEADS)
    if use_rope:
        q, k = axial_rope(q), axial_rope(k)
    k = k * (ML_DQK ** -0.5)
    v = split_heads(v.astype(jnp.float32), ML_HEADS)
    g = gates.astype(jnp.float32).reshape(B, L, 2, 2, ML_HEADS)
    log_i = jnp.transpose(g[:, :, 0] + i_bias, (2, 0, 3, 1))
    log_f = jnp.transpose(jax.nn.log_sigmoid(g[:, :, 1] + f_bias), (2, 0, 3, 1))
    to_bhl = lambda t: jnp.moveaxis(t, 1, 2)
    return to_bhl(q), to_bhl(k), to_bhl(v), o, log_i, log_f


def _mlstm_direction(prep, d, state, emit):
    q, k, v, _, log_i, log_f = prep
    seq = (q, k, v, log_f[d], log_i[d])
    if d == 1:
        seq = tuple(jnp.flip(t, 2) for t in seq)
    h, st = mlstm_chunk_scan(*seq, state, emit)
    if emit and d == 1:
        h = jnp.flip(h, 2)
    return h, st


def _mlstm_output(h, o, norm_w, dtype):
    y = head_norm(jnp.moveaxis(h, 1, 2), ML_NORM_EPS) * norm_w
    return (y * jax.nn.sigmoid(o.astype(jnp.float32))).astype(dtype)


def mlstm_mixer(pc_c, pc_l, conv_w, conv_b, i_bias, f_bias, norm_w, need_ctx):
    prep_c = _mlstm_prep(pc_c, conv_w, conv_b, i_bias, f_bias, False)
    prep_l = _mlstm_prep(pc_l, conv_w, conv_b, i_bias, f_bias, True)
    B = pc_c.shape[0]
    state0 = (jnp.zeros((B, ML_HEADS, ML_DQK, ML_DV), jnp.float32),
              jnp.zeros((B, ML_HEADS, ML_DQK), jnp.float32),
              jnp.zeros((B, ML_HEADS), jnp.float32))
    hs_c, hs_l = [], []
    for d in range(2):
        h_cd, st = _mlstm_direction(prep_c, d, state0, need_ctx)
        h_ld, _ = _mlstm_direction(prep_l, d, st, True)
        hs_c.append(h_cd)
        hs_l.append(h_ld)
    y_l = _mlstm_output(hs_l[0] + hs_l[1], prep_l[3], norm_w, pc_l.dtype)
    y_c = _mlstm_output(hs_c[0] + hs_c[1], prep_c[3], norm_w, pc_c.dtype) if need_ctx else None
    return y_c, y_l


def token_mixers(p_ctx, p_lat, rpb, mu_prev, mu_next, w0, w2, a0, a2, g2, k_k, k_a, r_k, rw_ln_w, rw_ln_b,
                 conv_w, conv_b, i_bias, f_bias, ml_norm_w, need_ctx):
    cuts = [3 * NA_WIDTH, 3 * NA_WIDTH + RW_PROJ]
    pa_c, pb_c, pc_c = jnp.split(p_ctx, cuts, axis=-1)
    pa_l, pb_l, pc_l = jnp.split(p_lat, cuts, axis=-1)
    ya_c, ya_l = na_mixer(pa_c, pa_l, rpb, need_ctx)
    yb_c, yb_l = rwkv7_mixer(pb_c, pb_l, mu_prev, mu_next, w0, w2, a0, a2, g2, k_k, k_a, r_k, rw_ln_w, rw_ln_b, need_ctx)
    yc_c, yc_l = mlstm_mixer(pc_c, pc_l, conv_w, conv_b, i_bias, f_bias, ml_norm_w, need_ctx)
    y_lat = jnp.concatenate([ya_l, yb_l, yc_l], axis=-1)
    if not need_ctx:
        return None, y_lat
    return jnp.concatenate([ya_c, yb_c, yc_c], axis=-1), y_lat


def routed_experts(h, expert_id, weight, w_gate, w_up, w_down):
    T, D = h.shape
    A = expert_id.shape[0]
    n_exp = w_gate.shape[0]
    tok = jnp.repeat(jnp.arange(T, dtype=jnp.int32), TOP_K)
    order = jnp.argsort(expert_id)
    e_sorted = expert_id[order]
    counts = jnp.bincount(expert_id, length=n_exp)
    starts = jnp.cumsum(counts) - counts
    padded = (counts + MOE_BLOCK - 1) // MOE_BLOCK * MOE_BLOCK
    p_ends = jnp.cumsum(padded)
    dest = (p_ends - padded)[e_sorted] + jnp.arange(A, dtype=jnp.int32) - starts[e_sorted]
    n_blocks = -(-A // MOE_BLOCK) + n_exp
    n_slots = n_blocks * MOE_BLOCK
    slot_tok = jnp.full((n_slots,), T, jnp.int32).at[dest].set(tok[order])
    slot_w = jnp.zeros((n_slots,), h.dtype).at[dest].set(weight[order].astype(h.dtype))
    block_exp = jnp.minimum(jnp.searchsorted(p_ends, jnp.arange(n_blocks, dtype=jnp.int32) * MOE_BLOCK, side='right'), n_exp - 1)
    xb = jnp.concatenate([h, jnp.zeros((1, D), h.dtype)], axis=0)[slot_tok].reshape(n_blocks, MOE_BLOCK, D)

    def expert_block(args):
        xblk, e = args
        return jnp.dot(jax.nn.silu(jnp.dot(xblk, w_gate[e])) * jnp.dot(xblk, w_up[e]), w_down[e])

    yb = lax.map(expert_block, (xb, block_exp)).reshape(n_slots, D) * slot_w[:, None]
    return jax.ops.segment_sum(yb, slot_tok, num_segments=T + 1)[:T]


def hierarchical_moe(h, wg_r, bg_r, we_r, be_r, w_gate, w_up, w_down):
    T = h.shape[0]
    t_idx = jnp.arange(T, dtype=jnp.int32)
    g_logits = jnp.dot(h, wg_r).astype(jnp.float32) + bg_r
    g_sel = jnp.argmax(g_logits, axis=-1)
    g_w = jax.nn.softmax(g_logits, axis=-1)[t_idx, g_sel]
    e_logits = (jnp.dot(h, we_r).astype(jnp.float32) + be_r).reshape(T, N_GROUPS, EXPERTS_PER_GROUP)
    top_v, top_i = lax.top_k(e_logits[t_idx, g_sel], TOP_K)
    weights = jax.nn.softmax(top_v, axis=-1) * g_w[:, None]
    expert_id = (g_sel[:, None] * EXPERTS_PER_GROUP + top_i).reshape(-1).astype(jnp.int32)
    return routed_experts(h, expert_id, weights.reshape(-1), w_gate, w_up, w_down)


def setup_inputs(seed: int = 0) -> dict:
    key = jax.random.key(seed)
    keys = iter(jax.random.split(key, 48))
    D = D_MODEL
    beta = (8.0 * DEPTH) ** -0.25

    def nrm(shape, scale):
        return jax.random.normal(next(keys), shape, jnp.float32) * scale

    def unif(shape, lo, hi):
        return jax.random.uniform(next(keys), shape, jnp.float32, lo, hi)

    gate_offset = jnp.repeat(jnp.array([0.0, 0.0, 1.0, 0.0, 0.0, 1.0], jnp.float32), D)
    decay_base = jnp.linspace(-6.0, -1.0, RW_WIDTH, dtype=jnp.float32)
    fgate_base = jnp.linspace(3.0, 6.0, ML_HEADS, dtype=jnp.float32)
    return {
        'x': nrm((BATCH, SEQ, D), 1.0),
        'c': nrm((BATCH, D), 1.0),
        'ctx': nrm((BATCH, CTX_LEN, D), 1.0),
        'c_ctx': nrm((D,), 1.0),
        'w_mod': nrm((DEPTH, D, 6 * D), 0.3 * D ** -0.5),
        'b_mod': nrm((DEPTH, 6 * D), 0.05) + gate_offset,
        'w_in': nrm((DEPTH, D, IN_PROJ), D ** -0.5),
        'na_rpb': nrm((DEPTH, NA_HEADS, 2 * NA_KH - 1, 2 * NA_KW - 1), 0.1),
        'rw_mu_prev': unif((DEPTH, RW_PROJ), 0.0, 0.5),
        'rw_mu_next': unif((DEPTH, RW_PROJ), 0.0, 0.5),
        'rw_w0': decay_base + nrm((DEPTH, 2, RW_WIDTH), 0.1),
        'rw_w2': nrm((DEPTH, 2, RW_DECAY_LORA, RW_WIDTH), 0.1 * RW_DECAY_LORA ** -0.5),
        'rw_a0': nrm((DEPTH, 2, RW_WIDTH), 0.1),
        'rw_a2': nrm((DEPTH, 2, RW_AAA_LORA, RW_WIDTH), RW_AAA_LORA ** -0.5),
        'rw_g2': nrm((DEPTH, RW_GATE_LORA, RW_WIDTH), RW_GATE_LORA ** -0.5),
        'rw_k_k': 0.85 + nrm((DEPTH, RW_WIDTH), 0.02),
        'rw_k_a': 1.0 + nrm((DEPTH, RW_WIDTH), 0.02),
        'rw_r_k': nrm((DEPTH, RW_WIDTH), 0.1),
        'rw_ln_w': 1.0 + nrm((DEPTH, RW_WIDTH), 0.02),
        'rw_ln_b': nrm((DEPTH, RW_WIDTH), 0.02),
        'ml_conv_w': nrm((DEPTH, ML_CONV, 2 * ML_QK_WIDTH), ML_CONV ** -0.5),
        'ml_conv_b': nrm((DEPTH, 2 * ML_QK_WIDTH), 0.02),
        'ml_i_bias': nrm((DEPTH, 2, ML_HEADS), 0.1),
        'ml_f_bias': fgate_base + nrm((DEPTH, 2, ML_HEADS), 0.1),
        'ml_norm_w': 1.0 + nrm((DEPTH, ML_WIDTH), 0.02),
        'w_out': nrm((DEPTH, MIX_WIDTH, D), beta * MIX_WIDTH ** -0.5),
        'ln1_g': 1.0 + nrm((DEPTH, D), 0.02),
        'ln1_b': nrm((DEPTH, D), 0.02),
        'router_group_w': nrm((DEPTH, D, N_GROUPS), D ** -0.5),
        'router_group_b': nrm((DEPTH, N_GROUPS), 0.01),
        'router_expert_w': nrm((DEPTH, D, N_EXPERTS), D ** -0.5),
        'router_expert_b': nrm((DEPTH, N_EXPERTS), 0.01),
        'exp_w_gate': nrm((DEPTH, N_EXPERTS, D, D_EXPERT), D ** -0.5),
        'exp_w_up': nrm((DEPTH, N_EXPERTS, D, D_EXPERT), D ** -0.5),
        'exp_w_down': nrm((DEPTH, N_EXPERTS, D_EXPERT, D), beta * D_EXPERT ** -0.5),
        'ln2_g': 1.0 + nrm((DEPTH, D), 0.02),
        'ln2_b': nrm((DEPTH, D), 0.02),
    }


def reference(x, c, ctx, c_ctx, w_mod, b_mod, w_in, na_rpb, rw_mu_prev, rw_mu_next, rw_w0, rw_w2, rw_a0, rw_a2,
              rw_g2, rw_k_k, rw_k_a, rw_r_k, rw_ln_w, rw_ln_b, ml_conv_w, ml_conv_b, ml_i_bias, ml_f_bias, ml_norm_w,
              w_out, ln1_g, ln1_b, router_group_w, router_group_b, router_expert_w, router_expert_b,
              exp_w_gate, exp_w_up, exp_w_down, ln2_g, ln2_b):
    B, L, D = x.shape
    Lc = ctx.shape[1]
    alpha = (2.0 * DEPTH) ** 0.25
    for l in range(DEPTH):
        last = l == DEPTH - 1
        mod = jnp.dot(jax.nn.silu(c), w_mod[l]) + b_mod[l]
        sh1, sc1, g1, sh2, sc2, g2 = [m[:, None, :] for m in jnp.split(mod, 6, axis=-1)]
        n_ctx_mod = 2 if last else 6
        mod_c = jnp.split(jnp.dot(jax.nn.silu(c_ctx), w_mod[l][:, :n_ctx_mod * D]) + b_mod[l][:n_ctx_mod * D], n_ctx_mod)
        p_lat = jnp.dot(x * (1 + sc1) + sh1, w_in[l])
        p_ctx = jnp.dot(ctx * (1 + mod_c[1]) + mod_c[0], w_in[l])
        y_ctx, y_lat = token_mixers(p_ctx, p_lat, na_rpb[l], rw_mu_prev[l], rw_mu_next[l], rw_w0[l], rw_w2[l],
                                    rw_a0[l], rw_a2[l], rw_g2[l], rw_k_k[l], rw_k_a[l], rw_r_k[l], rw_ln_w[l],
                                    rw_ln_b[l], ml_conv_w[l], ml_conv_b[l], ml_i_bias[l], ml_f_bias[l],
                                    ml_norm_w[l], not last)
        x = layer_norm(alpha * x + g1 * jnp.dot(y_lat, w_out[l]), ln1_g[l], ln1_b[l])
        h2 = (x * (1 + sc2) + sh2).reshape(B * L, D)
        moe_args = (router_group_w[l], router_group_b[l], router_expert_w[l], router_expert_b[l],
                    exp_w_gate[l], exp_w_up[l], exp_w_down[l])
        if last:
            f = hierarchical_moe(h2, *moe_args)
        else:
            ctx = layer_norm(alpha * ctx + mod_c[2] * jnp.dot(y_ctx, w_out[l]), ln1_g[l], ln1_b[l])
            hc2 = (ctx * (1 + mod_c[4]) + mod_c[3]).reshape(B * Lc, D)
            f_all = hierarchical_moe(jnp.concatenate([hc2, h2], axis=0), *moe_args)
            ctx = layer_norm(alpha * ctx + mod_c[5] * f_all[:B * Lc].reshape(B, Lc, D), ln2_g[l], ln2_b[l])
            f = f_all[B * Lc:]
        x = layer_norm(alpha * x + g2 * f.reshape(B, L, D), ln2_g[l], ln2_b[l])
    return x
```

```python
import numpy as np
import concourse.bass as bass
import concourse.mybir as mybir
from concourse.bass_utils import run_bass_kernel_spmd
from concourse.alu_op_type import AluOpType as ALU
F32 = mybir.dt.float32
BF16 = mybir.dt.bfloat16
AF = mybir.ActivationFunctionType


class Sched:
    ENG = ('pe', 'dve', 'act', 'pool', 'sp')
    NDMA = {'sp': 44}

    def __init__(self, nc):
        self.nc = nc
        self.ops = {e: [] for e in self.ENG}
        self.last_w = {}
        self.readers = {}
        self.dma_n = {q: 0 for q in self.NDMA}
        self.dma_val = {}
        self.epoch = 0

    def _deps(self, eng, reads, writes):
        if not hasattr(self, 'hz'):
            self.hz = set()
        deps = set()
        for k in list(reads) + list(writes):
            w = self.last_w.get(k)
            if w is not None:
                deps.add(w)
        for k in writes:
            for r in self.readers.get(k, ()):
                deps.add(r)
        deps = {d for d in deps if not (d[0] == 'eng' and d[1] == eng and eng in ('dve', 'pe') and (d[1], d[2]) not in self.hz)}
        best = {}
        for d in deps:
            k = (d[0], d[1])
            if k not in best or d[2] > best[k][2]:
                best[k] = d
        return set(best.values())

    def _mark(self, me, reads, writes):
        for k in writes:
            self.last_w[k] = me
            self.readers[k] = []
        for k in reads:
            self.readers.setdefault(k, []).append(me)

    def op(self, eng, fn, reads=(), writes=(), hz=False):
        deps = self._deps(eng, reads, writes)
        idx = len(self.ops[eng])
        if hz:
            self.hz.add((eng, idx))
        self.ops[eng].append(dict(fn=fn, deps=deps, kind='c', epoch=self.epoch))
        self._mark(('eng', eng, idx), reads, writes)

    def dma(self, fn, reads=(), writes=(), q='sp'):
        deps = self._deps(q, reads, writes)
        j = self.dma_n[q]
        self.dma_n[q] += 1
        slot = (q, j % self.NDMA[q])
        prev = self.dma_val.get(slot, 0)
        val = prev + 16
        self.dma_val[slot] = val
        if prev:
            deps.add(('dma', slot, prev))
        self.ops[q].append(dict(fn=fn, deps=deps, kind='d', slot=slot, val=val))
        self._mark(('dma', slot, val), reads, writes)

    def _last_real(self, e):
        for i in range(len(self.ops[e]) - 1, -1, -1):
            if self.ops[e][i]['fn'] is not None and self.ops[e][i]['kind'] == 'c':
                return i
        return None

    def new_epoch(self):
        self.epoch += 1

    def barrier(self):
        deps_all = {('dma', slot, v) for slot, v in self.dma_val.items()}
        last = {e: self._last_real(e) for e in self.ENG}
        for e in self.ENG:
            deps = set(deps_all)
            for e2, i2 in last.items():
                if e2 != e and i2 is not None:
                    deps.add(('eng', e2, i2))
            self.ops[e].append(dict(fn=None, deps=deps, kind='w'))
        self.last_w = {}
        self.readers = {}

    def cc(self, fn, reads=(), writes=()):
        if not hasattr(self, 'ncc'):
            self.ncc = 0
        deps = self._deps('pool', reads, writes)
        slot = ('cc', 0)
        self.ncc += 1
        val = 16 * self.ncc
        self.dma_val[slot] = val
        self.ops['pool'].append(dict(fn=fn, deps=deps, kind='cc', slot=slot, val=val, epoch=self.epoch))
        self._mark(('dma', slot, val), reads, writes)

    def finish_wait(self, eng='sp'):
        deps = set()
        for slot, v in self.dma_val.items():
            deps.add(('dma', slot, v))
        for e in self.ENG:
            i2 = self._last_real(e)
            if e != eng and i2 is not None:
                deps.add(('eng', e, i2))
        self.ops[eng].append(dict(fn=None, deps=deps, kind='w'))

    def emit(self, block, sems):
        needed = set()
        for e in self.ENG:
            for o in self.ops[e]:
                for d in o['deps']:
                    if d[0] == 'eng':
                        needed.add((d[1], d[2]))
        sig = {}
        for e in self.ENG:
            c = {}
            for i, o in enumerate(self.ops[e]):
                if (e, i) in needed:
                    ep = o['epoch']
                    c[ep] = c.get(ep, 0) + 1
                    sig[(e, i)] = c[ep]
        self.maxsig = {e: max([v for (ee, i), v in sig.items() if ee == e], default=0) for e in self.ENG}
        print('maxsig', self.maxsig, 'maxdma', max(self.dma_val.values()), 'nsem', len(self.sem_names()), 'nops', {e: len(self.ops[e]) for e in self.ENG}, flush=True)

        def run(e, engobj):
            waited = {}
            for i, o in enumerate(self.ops[e]):
                for d in o['deps']:
                    if d[0] == 'eng':
                        sname, v = ('E', d[1], self.ops[d[1]][d[2]]['epoch']), sig[(d[1], d[2])]
                    else:
                        sname, v = ('D', d[1]), d[2]
                    if waited.get(sname, 0) >= v:
                        continue
                    waited[sname] = v
                    engobj.wait_ge(sems[sname], v)
                if o['fn'] is None:
                    continue
                ins = o['fn'](engobj)
                if o['kind'] == 'd':
                    ins.then_inc(sems[('D', o['slot'])], 16)
                elif o['kind'] == 'cc':
                    ins.then_inc(sems[('D', o['slot'])], 16)
                elif (e, i) in needed:
                    ins.then_inc(sems[('E', e, o['epoch'])], 1)

        if self.ops['sp']:
            @block.sync
            def _(eng):
                run('sp', eng)
        if self.ops['pe']:
            @block.tensor
            def _(eng):
                run('pe', eng)
        if self.ops['dve']:
            @block.vector
            def _(eng):
                run('dve', eng)
        if self.ops['act']:
            @block.scalar
            def _(eng):
                run('act', eng)
        if self.ops['pool']:
            @block.gpsimd
            def _(eng):
                run('pool', eng)

    def sem_names(self):
        names = [('E', e, ep) for e in self.ENG if e != 'sp' for ep in range(self.epoch + 1)]
        for q, n in self.NDMA.items():
            for j in range(n):
                names.append(('D', (q, j)))
        if getattr(self, 'ncc', 0):
            names.append(('D', ('cc', 0)))
        return names


from contextlib import ExitStack

_UNIQ = [0]


class Scope:
    def __init__(self, nc, S):
        self.nc, self.S = nc, S
    def __enter__(self):
        self.st = ExitStack()
        self.st.__enter__()
        def sb(name, shape, dt=F32):
            _UNIQ[0] += 1
            return self.st.enter_context(self.nc.sbuf_tensor(f"{name}_u{_UNIQ[0]}", list(shape), dt))
        return sb
    def __exit__(self, *a):
        self.S.barrier()
        return self.st.__exit__(*a)


def build_program(body):
    nc = bass.Bass("TRN2", target_bir_lowering=False)
    with ExitStack() as st:
        S = Sched(nc)
        def sb(name, shape, dt=F32):
            return st.enter_context(nc.sbuf_tensor(name, list(shape), dt))
        def ps(name, shape, dt=F32):
            return st.enter_context(nc.psum_tensor(name, list(shape), dt))
        body(nc, S, sb, ps)
        S.finish_wait('sp')
        sems = {}
        for nm in S.sem_names():
            sems[nm] = st.enter_context(nc.semaphore("s_" + "_".join(str(x) for x in (nm[1] if isinstance(nm[1], tuple) else (nm[1],))) + nm[0] + (str(nm[2]) if len(nm) > 2 else "")))
        block = st.enter_context(nc.Block())
        S.emit(block, sems)
    return nc


import numpy as np
NSH = 4160
def colmap(j):
    a = np.arange
    na = np.concatenate([128 * j + a(128), 512 + 128 * j + a(128), 1024 + 128 * j + a(128)])
    rb = 1536
    rw = np.concatenate([rb + 64 * j + a(64), rb + 256 + 64 * j + a(64), rb + 512 + 64 * j + a(64), rb + 768 + a(64), rb + 832 + a(64), rb + 896 + a(64)])
    mb = 2496
    ml = np.concatenate([mb + 64 * j + a(64), mb + 256 + 64 * j + a(64), mb + 512 + 64 * j + a(64), mb + 768 + 64 * j + a(64),
                         mb + 1024 + np.array([j, 4 + j, 8 + j, 12 + j])])
    cols_c = np.concatenate([na, rw, ml])
    cols_n = np.concatenate([1024 + 128 * j + a(128), mb + 512 + 64 * j + a(64), mb + 768 + 64 * j + a(64)])
    return cols_c, cols_n
def x_full_of(xb, ctxb):
    return np.ascontiguousarray(np.concatenate([np.concatenate([ctxb[64 * k:64 * k + 64], xb[4096 * k:4096 * (k + 1)]], 0) for k in range(4)], 0))
def ident2():
    return np.stack([np.eye(128, dtype=np.float32), np.eye(128, dtype=np.float32)[::-1].copy()])
def seq_of_full(full):
    f = full.reshape(4, NSH, -1)
    return np.concatenate([f[:, :64].reshape(256, -1), f[:, 64:].reshape(16384, -1)], 0)


import numpy as np

D = 1024
TT = 16640
NTILE = 130
NCOL = 1028
NSH = 4160
ALPHA = 4.0 ** 0.25

def tile_src(view, vt):
    if vt < 2:
        ot = vt if view == 0 else 1 - vt
        segs = [((2 * ot) * NSH, 64), ((2 * ot + 1) * NSH, 64)]
    else:
        lt = vt - 2
        ot = lt if view == 0 else 127 - lt
        k, r = ot // 32, ot % 32
        segs = [(k * NSH + 64 + r * 128, 128)]
    return segs, (view == 1)


def ytile_dst(vt):
    if vt < 2:
        return [(2 * vt, 0, 64, 0), (2 * vt + 1, 0, 64, 64)]
    lt = vt - 2
    return [(lt // 32, 64 + (lt % 32) * 128, 128, 0)]


def stage1(nc, S, sb, P, dr, views=(0, 1)):
    x_full, cc, w_mod, b_modT, w_c, w_n, ident2 = dr['x_full'], dr['cc'], dr['w_mod'], dr['b_modT'], dr['w_c'], dr['w_n'], dr['ident2']
    pT, pN = dr['pT'], dr['pN']
    H = [sb(f"H{i}", [128, 8, 512], BF16) for i in range(2)]
    W = sb("W", [128, 8, NCOL], BF16)
    WN = sb("WN", [128, 8, 256], BF16)
    idj = sb("idj", [128, 2, 128])
    cct = sb("cct", [128, 16])
    sil = sb("sil", [128, 8, 2])
    bmt = sb("bmt", [128, 48])
    modT = sb("modT_sb", [128, 48, 2])
    sc1 = sb("sc1", [128, 8, 2])
    wst = [sb(f"wst{i}", [128, 8, 512]) for i in range(2)]
    xt = [sb(f"xt{i}", [128, D]) for i in range(3)]
    ot = [sb(f"ot{i}", [128, 512]) for i in range(4)]

    S.dma(lambda e: e.dma_start(out=idj[:], in_=ident2.rearrange("j p n -> p j n")), writes=['idj'])
    S.dma(lambda e: e.dma_start(out=cct[:], in_=cc[:, :]), writes=['cct'])
    S.dma(lambda e: e.dma_start(out=bmt[:], in_=b_modT[:, :]), writes=['bmt'])
    for j in range(2):
        S.op('act', lambda e, j=j: e.activation(out=sil[:, :, j], in_=cct[:, j * 8:(j + 1) * 8], func=AF.Silu),
             reads=['cct'], writes=['sil'])
    wm = w_mod.rearrange("(kc p) n -> p kc n", p=128)
    for j in range(12):
        buf = wst[j % 2]
        S.dma(lambda e, j=j, buf=buf: e.dma_start(out=buf[:], in_=wm[:, :, j * 512:(j + 1) * 512]), writes=[f'wst{j%2}'])
        for c4 in range(4):
            col = j * 4 + c4
            pt = P[col % 2]
            for kc in range(8):
                S.op('pe', lambda e, buf=buf, kc=kc, c4=c4, pt=pt: e.matmul(
                    pt[:, 0:2], lhsT=buf[:, kc, c4 * 128:(c4 + 1) * 128], rhs=sil[:, kc, :],
                    start=(kc == 0), stop=(kc == 7)), reads=[f'wst{j%2}', 'sil'], writes=[f'P{col%2}'])
            S.op('dve', lambda e, col=col, pt=pt: e.tensor_scalar(
                out=modT[:, col, :], in0=pt[:, 0:2], scalar1=bmt[:, col:col + 1], scalar2=None, op0=ALU.add),
                reads=[f'P{col%2}', 'bmt'], writes=['modT'])
    S.op('dve', lambda e: e.tensor_scalar(out=sc1[:], in0=modT[:, 8:16, :], scalar1=1.0, scalar2=None, op0=ALU.add),
         reads=['modT'], writes=['sc1'])
    if 'modT_out' in dr:
        S.dma(lambda e: e.dma_start(out=dr['modT_out'].rearrange("p (c j) -> p c j", j=2), in_=modT[:]), reads=['modT'])
    wi = w_c.rearrange("(kc p) n -> p kc n", p=128)
    nj = (NCOL + 511) // 512
    for j in range(nj):
        cw = min(512, NCOL - j * 512)
        buf = wst[j % 2]
        S.dma(lambda e, j=j, buf=buf, cw=cw: e.dma_start(out=buf[:, :, 0:cw], in_=wi[:, :, j * 512:j * 512 + cw]), writes=[f'wst{j%2}'])
        S.op('pool', lambda e, j=j, buf=buf, cw=cw: e.tensor_copy(out=W[:, :, j * 512:j * 512 + cw], in_=buf[:, :, 0:cw]),
             reads=[f'wst{j%2}'], writes=['W'])
    wn = w_n.rearrange("(kc p) n -> p kc n", p=128)
    buf = wst[nj % 2]
    S.dma(lambda e, buf=buf: e.dma_start(out=buf[:, :, 0:256], in_=wn[:, :, :]), writes=[f'wst{nj%2}'])
    S.op('pool', lambda e, buf=buf: e.tensor_copy(out=WN[:], in_=buf[:, :, 0:256]), reads=[f'wst{nj%2}'], writes=['WN'])

    it = 0
    xi = 0
    pi_t = 0
    for view in views:
        chunks = list(range(9)) if view == 0 else list(range(3, 9))
        for blk in range(33):
            ntile = 2 if blk == 0 else 4
            vt0 = 0 if blk == 0 else 2 + (blk - 1) * 4
            s0 = vt0 * 128
            tw = ntile * 128
            Hb = H[blk % 2]
            hk = f'H{blk%2}'
            mj = 1 if blk == 0 else 0
            for tl in range(ntile):
                vt = vt0 + tl
                segs, rev = tile_src(view, vt)
                xb = xt[xi % 3]
                xk = f'xt{xi%3}'
                xi += 1
                r = 0
                for (row0, n) in segs:
                    S.dma(lambda e, xb=xb, r=r, n=n, row0=row0: e.dma_start(out=xb[r:r + n, :], in_=x_full[row0:row0 + n, :]), writes=[xk])
                    r += n
                for half in range(2):
                    pslot = 2 + pi_t % 4
                    pi_t += 1
                    pt = P[pslot]
                    for k4 in range(4):
                        kc = half * 4 + k4
                        S.op('pe', lambda e, xb=xb, kc=kc, k4=k4, pt=pt, rev=rev: e.matmul(
                            pt[:, k4 * 128:(k4 + 1) * 128], lhsT=xb[:, kc * 128:(kc + 1) * 128], rhs=idj[:, 1 if rev else 0, :],
                            start=True, stop=True), reads=[xk, 'idj'], writes=[f'P{pslot}'])
                    for k4 in range(4):
                        kc = half * 4 + k4
                        S.op('act', lambda e, kc=kc, k4=k4, pt=pt, tl=tl, Hb=Hb, mj=mj: e.activation(
                            out=Hb[:, kc, tl * 128:(tl + 1) * 128], in_=pt[:, k4 * 128:(k4 + 1) * 128], func=AF.Identity,
                            bias=modT[:, kc, mj:mj + 1], scale=sc1[:, kc, mj:mj + 1]),
                            reads=[f'P{pslot}', 'modT', 'sc1'], writes=[hk])
            for cc_ in chunks:
                c0 = cc_ * 128
                cw = min(128, NCOL - c0)
                pslot = 6 + it % 2
                pt = P[pslot]
                for kc in range(8):
                    S.op('pe', lambda e, kc=kc, c0=c0, cw=cw, tw=tw, pt=pt, Hb=Hb: e.matmul(
                        pt[0:cw, 0:tw], lhsT=W[:, kc, c0:c0 + cw], rhs=Hb[:, kc, 0:tw],
                        start=(kc == 0), stop=(kc == 7)), reads=['W', hk], writes=[f'P{pslot}'])
                ob = ot[it % 4]
                okey = f'ot{it%4}'
                if it % 2 == 0:
                    S.op('dve', lambda e, ob=ob, pt=pt, cw=cw, tw=tw: e.tensor_copy(out=ob[0:cw, 0:tw], in_=pt[0:cw, 0:tw]),
                         reads=[f'P{pslot}'], writes=[okey])
                else:
                    S.op('act', lambda e, ob=ob, pt=pt, cw=cw, tw=tw: e.copy(out=ob[0:cw, 0:tw], in_=pt[0:cw, 0:tw]),
                         reads=[f'P{pslot}'], writes=[okey])
                S.dma(lambda e, ob=ob, c0=c0, cw=cw, s0=s0, tw=tw, view=view: e.dma_start(
                    out=pT[view, c0:c0 + cw, s0:s0 + tw], in_=ob[0:cw, 0:tw]), reads=[okey])
                it += 1
            for tp in range(ntile // 2):
                pslot = 6 + it % 2
                pt = P[pslot]
                for t2 in range(2):
                    tl = tp * 2 + t2
                    for kc in range(8):
                        S.op('pe', lambda e, kc=kc, tl=tl, t2=t2, pt=pt, Hb=Hb: e.matmul(
                            pt[:, t2 * 256:(t2 + 1) * 256], lhsT=Hb[:, kc, tl * 128:(tl + 1) * 128], rhs=WN[:, kc, :],
                            start=(kc == 0), stop=(kc == 7)), reads=['WN', hk], writes=[f'P{pslot}'])
                ob = ot[it % 4]
                okey = f'ot{it%4}'
                S.op('dve', lambda e, ob=ob, pt=pt: e.tensor_copy(out=ob[:, :], in_=pt[:, :]), reads=[f'P{pslot}'], writes=[okey])
                r0 = s0 + tp * 256
                S.dma(lambda e, ob=ob, r0=r0, view=view: e.dma_start(
                    out=pN[view, r0:r0 + 256, :].rearrange("(t p) c -> p t c", p=128),
                    in_=ob[:, :].rearrange("p (t c) -> p t c", c=256)), reads=[okey])
                it += 1


def na_configs():
    rows, W_, kh, kw = 256, 64, 8, 16
    MASK = 15 * 31
    cfgs = {}
    cfg_list = []
    sched = []
    qc = np.arange(64)
    cs = np.clip(qc - kw // 2, 0, W_ - kw)
    for i in range(128):
        qr = np.array([2 * i, 2 * i + 1])
        rs = np.clip(qr - kh // 2, 0, rows - kh)
        kts = sorted(set((rs.min() + np.arange(0, rs.max() + kh - rs.min())) // 2))
        lst = []
        for kt in kts:
            idx = np.full((128, 128), MASK, np.int32)
            for a in range(2):
                kr = 2 * kt + a
                for b in range(2):
                    r = qr[b]
                    if not (rs[b] <= kr < rs[b] + kh):
                        continue
                    kc = np.arange(64)[:, None]
                    q = qc[None, :]
                    valid = (kc >= cs[None, :]) & (kc < cs[None, :] + kw)
                    v = (kr - r + 7) * 31 + (kc - q + 15)
                    blk = np.where(valid, v, MASK)
                    idx[a * 64:(a + 1) * 64, b * 64:(b + 1) * 64] = blk
            if (idx == MASK).all():
                continue
            key = idx.tobytes()
            if key not in cfgs:
                cfgs[key] = len(cfg_list)
                cfg_list.append(idx)
            lst.append((int(kt), cfgs[key]))
        sched.append(lst)
    return np.stack(cfg_list), sched


NA_CFG, NA_SCHED = na_configs()
NCFG = NA_CFG.shape[0]


def na_bias_tables(rpb_h2):
    out = np.empty((2, 128, NCFG, 128), np.float32)
    for hh in range(2):
        ext = np.concatenate([rpb_h2[hh].reshape(-1), np.array([-10000.0], np.float32)])
        out[hh] = ext[NA_CFG].transpose(1, 0, 2)
    return out


def stage_na(nc, S, sb, P, dr):
    pT, pN, biasT, yT = dr['pT'], dr['pN'], dr['na_bias'], dr['yT']
    QT = sb("na_QT", [64, TT], BF16)
    KT = sb("na_KT", [64, TT], BF16)
    V = sb("na_V", [128, NTILE, 64], BF16)
    stg = [sb(f"na_stg{i}", [128, 2080]) for i in range(2)]
    bias = sb("na_bias_sb", [128, NCFG, 128])
    ones = sb("na_ones", [128, 64], BF16)
    tmp = [sb(f"na_tmp{i}", [128, 128]) for i in range(3)]
    PTb = [sb(f"na_PT{i}", [128, 128], BF16) for i in range(4)]
    rden = [sb(f"na_rden{i}", [64, 128]) for i in range(2)]
    yt = [sb(f"na_yt{i}", [64, 128]) for i in range(2)]
    S.op('dve', lambda e: e.memset(ones[:], 1.0), writes=['na_ones'])
    si = 0
    for hh in range(2):
        S.dma(lambda e, hh=hh: e.dma_start(out=bias[:], in_=biasT[hh]), writes=['na_bias'])
        for which, dst, row0 in (('q', QT, hh * 64), ('k', KT, 128 + hh * 64)):
            for pc in range(8):
                st_ = stg[si % 2]; sk = f'na_stg{si%2}'; si += 1
                S.dma(lambda e, st_=st_, row0=row0, pc=pc: e.dma_start(out=st_[0:64, :], in_=pT[0, row0:row0 + 64, pc * 2080:(pc + 1) * 2080]), writes=[sk])
                S.op('pool', lambda e, st_=st_, dst=dst, pc=pc: e.tensor_copy(out=dst[:, pc * 2080:(pc + 1) * 2080], in_=st_[0:64, :]),
                     reads=[sk], writes=['na_' + which])
        for pc in range(5):
            st_ = stg[si % 2]; sk = f'na_stg{si%2}'; si += 1
            S.dma(lambda e, st_=st_, pc=pc, hh=hh: e.dma_start(
                out=st_[:, 0:26 * 64].rearrange("p (t d) -> p t d", d=64),
                in_=pN[0, pc * 26 * 128:(pc + 1) * 26 * 128, hh * 64:(hh + 1) * 64].rearrange("(t p) d -> p t d", p=128)), writes=[sk])
            S.op('pool', lambda e, st_=st_, pc=pc: e.tensor_copy(out=V[:, pc * 26:(pc + 1) * 26, :], in_=st_[:, 0:26 * 64].rearrange("p (t d) -> p t d", d=64)),
                 reads=[sk], writes=['na_v'])
        slot = 0
        for vt in range(NTILE):
            par = vt % 2
            Ob, Db = P[4 + par], P[6 + par]
            if vt < 2:
                keys = [(0, None), (1, None)]
            else:
                keys = [(kt + 2, cfg) for (kt, cfg) in NA_SCHED[vt - 2]] + [(0, None), (1, None)]
            nk = len(keys)
            for ki, (kvt, cfg) in enumerate(keys):
                s = slot % 8; slot += 1
                Sb = P[s // 4][:, (s % 4) * 128:(s % 4 + 1) * 128]
                skey = f'naS{s}'
                S.op('pe', lambda e, Sb=Sb, kvt=kvt, vt=vt: e.matmul(Sb, lhsT=KT[:, kvt * 128:(kvt + 1) * 128], rhs=QT[:, vt * 128:(vt + 1) * 128],
                                                                   start=True, stop=True), reads=['na_q', 'na_k'], writes=[skey])
                pb = PTb[s % 4]; pk = f'naPT{s%4}'
                if cfg is not None:
                    tb = tmp[s % 3]; tk = f'natmp{s%3}'
                    S.op('dve', lambda e, tb=tb, Sb=Sb, cfg=cfg: e.scalar_tensor_tensor(out=tb[:], in0=Sb, scalar=0.125, in1=bias[:, cfg, :], op0=ALU.mult, op1=ALU.add),
                         reads=[skey, 'na_bias'], writes=[tk])
                    S.op('act', lambda e, tb=tb, pb=pb: e.activation(out=pb[:], in_=tb[:], func=AF.Exp), reads=[tk], writes=[pk])
                else:
                    S.op('act', lambda e, Sb=Sb, pb=pb: e.activation(out=pb[:], in_=Sb, func=AF.Exp, scale=0.125), reads=[skey], writes=[pk])
                S.op('pe', lambda e, Ob=Ob, pb=pb, kvt=kvt, ki=ki, nk=nk: e.matmul(Ob[0:64, 0:128], lhsT=V[:, kvt, :], rhs=pb[:], start=(ki == 0), stop=(ki == nk - 1)),
                     reads=[pk, 'na_v'], writes=[f'naO{par}'])
                S.op('pe', lambda e, Db=Db, pb=pb, ki=ki, nk=nk: e.matmul(Db[0:64, 0:128], lhsT=ones[:], rhs=pb[:], start=(ki == 0), stop=(ki == nk - 1)),
                     reads=[pk, 'na_ones'], writes=[f'naD{par}'])
            rd = rden[par]; yb = yt[par]
            S.op('dve', lambda e, rd=rd, Db=Db: e.reciprocal(out=rd[:], in_=Db[0:64, 0:128]), reads=[f'naD{par}'], writes=[f'na_rden{par}'])
            S.op('dve', lambda e, rd=rd, yb=yb, Ob=Ob: e.tensor_tensor(out=yb[:], in0=Ob[0:64, 0:128], in1=rd[:], op=ALU.mult),
                 reads=[f'naO{par}', f'na_rden{par}'], writes=[f'na_yt{par}'])
            for (k, m0, n, c0) in ytile_dst(vt):
                S.dma(lambda e, yb=yb, k=k, m0=m0, n=n, c0=c0, hh=hh: e.dma_start(out=yT[k, hh * 64:(hh + 1) * 64, m0:m0 + n], in_=yb[:, c0:c0 + n]),
                      reads=[f'na_yt{par}'])


RW0 = 384
RW_NPAR = 24

def rw_params(inp, l, j):
    mp, mn = inp['rw_mu_prev'][l], inp['rw_mu_next'][l]
    par = np.zeros((128, RW_NPAR), np.float32)
    ch = np.arange(64)
    for qi, off in enumerate((0, 256, 512)):
        cols = off + 64 * j + ch
        par[0:64, 2 * qi] = mp[cols]; par[0:64, 2 * qi + 1] = mn[cols]
        par[64:128, 2 * qi] = mn[cols]; par[64:128, 2 * qi + 1] = mp[cols]
    for qi, off in ((3, 768), (4, 832)):
        c0_ = off + np.arange(32); c1_ = off + 32 + np.arange(32)
        par[0:32, 2 * qi] = mp[c0_]; par[0:32, 2 * qi + 1] = mn[c0_]
        par[32:64, 2 * qi] = mn[c1_]; par[32:64, 2 * qi + 1] = mp[c1_]
    cg = 896 + ch
    par[0:64, 10] = mp[cg]; par[0:64, 11] = mn[cg]
    hc = 64 * j + ch
    for v in range(2):
        par[v * 64:(v + 1) * 64, 12] = inp['rw_w0'][l][v, hc]
        par[v * 64:(v + 1) * 64, 13] = inp['rw_a0'][l][v, hc]
        par[v * 64:(v + 1) * 64, 14] = inp['rw_k_k'][l][hc]
        par[v * 64:(v + 1) * 64, 15] = inp['rw_k_a'][l][hc]
        par[v * 64:(v + 1) * 64, 16] = inp['rw_r_k'][l][hc]
        par[v * 64:(v + 1) * 64, 17] = inp['rw_ln_w'][l][hc]
        par[v * 64:(v + 1) * 64, 18] = inp['rw_ln_b'][l][hc]
    mats = np.zeros((3, 64, 128), np.float32)
    for v in range(2):
        mats[0, v * 32:(v + 1) * 32, v * 64:(v + 1) * 64] = inp['rw_w2'][l][v][:, hc]
        mats[1, v * 32:(v + 1) * 32, v * 64:(v + 1) * 64] = inp['rw_a2'][l][v][:, hc]
    mats[2, :, 0:64] = inp['rw_g2'][l][:, hc]
    return par, mats


def rw_consts():
    c = np.zeros((3, 128, 128), np.float32)
    c[0, 0:64, 0:64] = 1.0; c[0, 64:128, 64:128] = 1.0
    c[1, 0, 0:64] = 1.0; c[1, 1, 64:128] = 1.0
    return c


def stage_rw(nc, S, sb, P, dr, Scope_):
    pT, yT, F, BG = dr['pT'], dr['yT'], dr['rwF'], dr['rwBG']
    par = sb("rw_par_sb", [128, RW_NPAR])
    mats = sb("rw_mats_sb", [64, 3, 128])
    cst = sb("rw_cst_sb", [128, 3, 128])
    idj = sb("rw_idj", [128, 2, 128])
    V = sb("rw_V", [128, TT])
    Y = sb("rw_Y", [128, TT])
    S.dma(lambda e: e.dma_start(out=par[:], in_=dr['rw_par'][:, :]), writes=['rw_par'])
    S.dma(lambda e: e.dma_start(out=mats[:], in_=dr['rw_mats'].rearrange("m p n -> p m n")), writes=['rw_mats'])
    S.dma(lambda e: e.dma_start(out=cst[:], in_=dr['rw_cst'].rearrange("m p n -> p m n")), writes=['rw_cst'])
    S.dma(lambda e: e.dma_start(out=idj[:], in_=dr['ident2'].rearrange("j p n -> p j n")), writes=['rw_idj'])
    c0t = sb("rw_c0", [128, 8])
    for qi in range(6):
        S.op('dve', lambda e, qi=qi: e.tensor_tensor(out=c0t[:, qi:qi + 1], in0=par[:, 2 * qi:2 * qi + 1], in1=par[:, 2 * qi + 1:2 * qi + 2], op=ALU.add),
             reads=['rw_par'], writes=['rw_c0'], hz=True)
    S.op('dve', lambda e: e.tensor_scalar(out=c0t[:, 0:6], in0=c0t[:, 0:6], scalar1=-1.0, scalar2=1.0, op0=ALU.mult, op1=ALU.add),
         reads=['rw_c0'], writes=['rw_c0'], hz=True)
    S.op('dve', lambda e: e.tensor_scalar(out=c0t[:, 6:7], in0=par[:, 15:16], scalar1=-1.0, scalar2=1.0, op0=ALU.mult, op1=ALU.add),
         reads=['rw_par', 'rw_c0'], writes=['rw_c0'], hz=True)
    blockones = cst[:, 0, :]
    sel2 = cst[0:2, 1, :]

    with Scope_(nc, S) as sb2:
        NBM = 512
        X = {q: sb2(f"rwX{q}", [128, NBM + 2]) for q in range(6)}
        xs = {q: sb2(f"rwxs{q}", [128, NBM]) for q in range(6)}
        tA = [sb2(f"rwt{i}", [128, NBM]) for i in range(10)]
        stg = [sb2(f"rwstg{i}", [128, 4, 128]) for i in range(3)]
        sti = 0
        for blk in range(33):
            s0 = 0 if blk == 0 else 256 + (blk - 1) * 512
            NB = 256 if blk == 0 else 512
            lz = (blk == 0 or blk == 1)
            rz = (blk == 0 or blk == 32)
            lo = s0 - (0 if lz else 1)
            hi = s0 + NB + (0 if rz else 1)
            c_lo = 1 if lz else 0
            for q in range(6):
                Xq = X[q]
                xk = f'rwX{q}'
                if lz:
                    S.op('pool', lambda e, Xq=Xq: e.memset(Xq[:, 0:1], 0.0), writes=[xk])
                if rz:
                    S.op('pool', lambda e, Xq=Xq, NB=NB: e.memset(Xq[:, NB + 1:NB + 2], 0.0), writes=[xk])
                if q < 3:
                    for v in range(2):
                        S.dma(lambda e, Xq=Xq, q=q, v=v, lo=lo, hi=hi, c_lo=c_lo: e.dma_start(
                            out=Xq[v * 64:(v + 1) * 64, c_lo:c_lo + hi - lo], in_=pT[v, RW0 + q * 64:RW0 + (q + 1) * 64, lo:hi]), writes=[xk])
                elif q < 5:
                    for v in range(2):
                        r0 = RW0 + 192 + (q - 3) * 64 + v * 32
                        S.dma(lambda e, Xq=Xq, v=v, r0=r0, lo=lo, hi=hi, c_lo=c_lo: e.dma_start(
                            out=Xq[v * 32:(v + 1) * 32, c_lo:c_lo + hi - lo], in_=pT[v, r0:r0 + 32, lo:hi]), writes=[xk])
                else:
                    S.dma(lambda e, Xq=Xq, lo=lo, hi=hi, c_lo=c_lo: e.dma_start(
                        out=Xq[0:64, c_lo:c_lo + hi - lo], in_=pT[0, RW0 + 320:RW0 + 384, lo:hi]), writes=[xk])
            for q in range(6):
                np_ = 128 if q < 3 else 64
                Xq, xq = X[q], xs[q]
                xk, sk = f'rwX{q}', f'rwxs{q}'
                eng = 'dve' if q % 2 == 0 else 'pool'
                S.op(eng, lambda e, Xq=Xq, xq=xq, q=q, np_=np_, NB=NB: e.tensor_scalar(
                    out=xq[0:np_, 0:NB], in0=Xq[0:np_, 1:NB + 1], scalar1=c0t[0:np_, q:q + 1], scalar2=None, op0=ALU.mult),
                    reads=[xk, 'rw_c0'], writes=[sk])
                S.op('dve', lambda e, Xq=Xq, xq=xq, q=q, np_=np_, NB=NB: e.scalar_tensor_tensor(
                    out=xq[0:np_, 0:NB], in0=Xq[0:np_, 0:NB], scalar=par[0:np_, 2 * q:2 * q + 1], in1=xq[0:np_, 0:NB], op0=ALU.mult, op1=ALU.add),
                    reads=[xk, sk, 'rw_par'], writes=[sk])
                S.op('dve', lambda e, Xq=Xq, xq=xq, q=q, np_=np_, NB=NB: e.scalar_tensor_tensor(
                    out=xq[0:np_, 0:NB], in0=Xq[0:np_, 2:NB + 2], scalar=par[0:np_, 2 * q + 1:2 * q + 2], in1=xq[0:np_, 0:NB], op0=ALU.mult, op1=ALU.add),
                    reads=[xk, sk, 'rw_par'], writes=[sk])
            xr, xk_, xv, xwd, xad, xgd = [xs[q] for q in range(6)]
            S.op('pool', lambda e, s0=s0, NB=NB: e.tensor_copy(out=V[:, s0:s0 + NB], in_=xv[:, 0:NB]), reads=['rwxs2'], writes=['rw_V'])
            wdT, sg, dec, av, kk, kk2, rn, kkn, tq, outq = tA
            S.op('act', lambda e, NB=NB: e.activation(out=wdT[0:64, 0:NB], in_=xwd[0:64, 0:NB], func=AF.Tanh), reads=['rwxs3'], writes=['rwt0'])
            S.op('pe', lambda e, NB=NB: e.matmul(P[0][:, 0:NB], lhsT=mats[:, 0, :], rhs=wdT[0:64, 0:NB], start=True, stop=True),
                 reads=['rwt0', 'rw_mats'], writes=['P0'])
            S.op('act', lambda e, NB=NB: e.activation(out=sg[:, 0:NB], in_=P[0][:, 0:NB], func=AF.Sigmoid, bias=par[:, 12:13], scale=1.0),
                 reads=['P0', 'rw_par'], writes=['rwt1'])
            S.op('act', lambda e, NB=NB: e.activation(out=dec[:, 0:NB], in_=sg[:, 0:NB], func=AF.Exp, scale=-0.6065306597126334),
                 reads=['rwt1'], writes=['rwt2'])
            S.op('pe', lambda e, NB=NB: e.matmul(P[1][:, 0:NB], lhsT=mats[:, 1, :], rhs=xad[0:64, 0:NB], start=True, stop=True),
                 reads=['rwxs4', 'rw_mats'], writes=['P1'])
            S.op('act', lambda e, NB=NB: e.activation(out=av[:, 0:NB], in_=P[1][:, 0:NB], func=AF.Sigmoid, bias=par[:, 13:14], scale=1.0),
                 reads=['P1', 'rw_par'], writes=['rwt3'])
            S.op('dve', lambda e, NB=NB: e.tensor_scalar(out=kk[:, 0:NB], in0=xk_[:, 0:NB], scalar1=par[:, 14:15], scalar2=None, op0=ALU.mult),
                 reads=['rwxs1', 'rw_par'], writes=['rwt4'])
            S.op('pool', lambda e, NB=NB: e.tensor_tensor(out=kk2[:, 0:NB], in0=kk[:, 0:NB], in1=kk[:, 0:NB], op=ALU.mult), reads=['rwt4'], writes=['rwt5'])
            S.op('pe', lambda e, NB=NB: e.matmul(P[2][:, 0:NB], lhsT=blockones, rhs=kk2[:, 0:NB], start=True, stop=True),
                 reads=['rwt5', 'rw_cst'], writes=['P2'])
            S.op('act', lambda e, NB=NB: e.activation(out=rn[:, 0:NB], in_=P[2][:, 0:NB], func=AF.Sqrt), reads=['P2'], writes=['rwt6'])
            S.op('dve', lambda e, NB=NB: e.tensor_scalar(out=rn[:, 0:NB], in0=rn[:, 0:NB], scalar1=1e-12, scalar2=None, op0=ALU.max), reads=['rwt6'], writes=['rwt6'])
            S.op('dve', lambda e, NB=NB: e.reciprocal(out=rn[:, 0:NB], in_=rn[:, 0:NB]), reads=['rwt6'], writes=['rwt6'])
            S.op('dve', lambda e, NB=NB: e.tensor_tensor(out=kkn[:, 0:NB], in0=kk[:, 0:NB], in1=rn[:, 0:NB], op=ALU.mult), reads=['rwt4', 'rwt6'], writes=['rwt7'])
            def emit_rows(qidx, src, skey):
                nonlocal sti
                nt = NB // 128
                pslot = 4 + sti % 3
                for tl in range(nt):
                    S.op('pe', lambda e, tl=tl, src=src, pslot=pslot: e.transpose(P[pslot][:, tl * 128:(tl + 1) * 128], src[:, tl * 128:(tl + 1) * 128], idj[:, 0, :]),
                         reads=[skey, 'rw_idj'], writes=[f'P{pslot}'])
                sg_ = stg[sti % 3]; gk = f'rwstg{sti%3}'
                eng = 'act' if sti % 2 == 0 else 'dve'
                if eng == 'act':
                    S.op('act', lambda e, sg_=sg_, pslot=pslot, nt=nt: e.copy(out=sg_[:, 0:nt, :], in_=P[pslot][:, 0:nt * 128].rearrange("p (t c) -> p t c", c=128)),
                         reads=[f'P{pslot}'], writes=[gk])
                else:
                    S.op('dve', lambda e, sg_=sg_, pslot=pslot, nt=nt: e.tensor_copy(out=sg_[:, 0:nt, :], in_=P[pslot][:, 0:nt * 128].rearrange("p (t c) -> p t c", c=128)),
                         reads=[f'P{pslot}'], writes=[gk])
                for v in range(2):
                    S.dma(lambda e, sg_=sg_, v=v, nt=nt, qidx=qidx, s0=s0: e.dma_start(
                        out=F[qidx, v, s0:s0 + nt * 128, :].rearrange("(t p) k -> p t k", p=128), in_=sg_[:, 0:nt, v * 64:(v + 1) * 64]), reads=[gk])
                sti += 1
            emit_rows(0, dec, 'rwt2')
            S.op('pool', lambda e, NB=NB: e.tensor_scalar(out=tq[:, 0:NB], in0=kkn[:, 0:NB], scalar1=-1.0, scalar2=None, op0=ALU.mult), reads=['rwt7'], writes=['rwt8'])
            emit_rows(1, tq, 'rwt8')
            S.op('dve', lambda e, NB=NB: e.tensor_tensor(out=outq[:, 0:NB], in0=kkn[:, 0:NB], in1=av[:, 0:NB], op=ALU.mult), reads=['rwt7', 'rwt3'], writes=['rwt9'])
            emit_rows(2, outq, 'rwt9')
            S.op('dve', lambda e, NB=NB: e.tensor_scalar(out=tq[:, 0:NB], in0=av[:, 0:NB], scalar1=par[:, 15:16], scalar2=c0t[:, 6:7], op0=ALU.mult, op1=ALU.add),
                 reads=['rwt3', 'rw_par', 'rw_c0'], writes=['rwt8'])
            S.op('dve', lambda e, NB=NB: e.tensor_tensor(out=outq[:, 0:NB], in0=tq[:, 0:NB], in1=xk_[:, 0:NB], op=ALU.mult), reads=['rwt8', 'rwxs1'], writes=['rwt9'])
            emit_rows(3, outq, 'rwt9')
            emit_rows(4, xr, 'rwxs0')
            S.op('dve', lambda e, NB=NB: e.scalar_tensor_tensor(out=tq[:, 0:NB], in0=xr[:, 0:NB], scalar=par[:, 16:17], in1=xk_[:, 0:NB], op0=ALU.mult, op1=ALU.mult),
                 reads=['rwxs0', 'rwxs1', 'rw_par'], writes=['rwt8'])
            S.op('pe', lambda e, NB=NB: e.matmul(P[3][:, 0:NB], lhsT=blockones, rhs=tq[:, 0:NB], start=True, stop=True), reads=['rwt8', 'rw_cst'], writes=['P3'])
            S.op('dve', lambda e, NB=NB: e.tensor_tensor(out=outq[0:64, 0:NB], in0=P[3][0:64, 0:NB], in1=xv[0:64, 0:NB], op=ALU.mult), reads=['P3', 'rwxs2'], writes=['rwt9'])
            S.dma(lambda e, NB=NB, s0=s0: e.dma_start(out=BG[0, :, s0:s0 + NB], in_=outq[0:64, 0:NB]), reads=['rwt9'])
            S.op('act', lambda e, NB=NB: e.activation(out=wdT[0:64, 0:NB], in_=xgd[0:64, 0:NB], func=AF.Sigmoid), reads=['rwxs5'], writes=['rwt0'])
            S.op('pe', lambda e, NB=NB: e.matmul(P[3][0:64, 0:NB], lhsT=mats[:, 2, 0:64], rhs=wdT[0:64, 0:NB], start=True, stop=True), reads=['rwt0', 'rw_mats'], writes=['P3'])
            S.op('act', lambda e, NB=NB: e.copy(out=sg[0:64, 0:NB], in_=P[3][0:64, 0:NB]), reads=['P3'], writes=['rwt1'])
            S.dma(lambda e, NB=NB, s0=s0: e.dma_start(out=BG[1, :, s0:s0 + NB], in_=sg[0:64, 0:NB]), reads=['rwt1'])

    with Scope_(nc, S) as sb3:
        NS = 16
        R = [sb3(f"rwR{i}", [2, 5, NS * 64]) for i in range(2)]
        St = sb3("rwS", [128, 64])
        junk = sb3("rwjunk", [128, 64])
        junk2 = sb3("rwjunk2", [128, 64])
        sa = sb3("rwsa", [128, 1])
        S.op('dve', lambda e: e.memset(St[:], 0.0), writes=['rwS'])
        nsb = TT // NS
        for b16 in range(nsb):
            Rb = R[b16 % 2]; rk_ = f'rwR{b16%2}'
            t0 = b16 * NS
            S.dma(lambda e, Rb=Rb, t0=t0: e.dma_start(out=Rb[:], in_=F[:, :, t0:t0 + NS, :].rearrange("q v t k -> v q (t k)")), writes=[rk_])
            for sub in range(NS // 4):
                st_ = (b16 * (NS // 4) + sub) % 2
                banks = [P[3 * st_ + 0], P[3 * st_ + 1], P[3 * st_ + 2]]
                pk = f'rwPS{st_}'
                views = []
                for q in range(5):
                    dst = banks[q // 2][:, (q % 2) * 256:(q % 2) * 256 + 256]
                    views.append(dst)
                    S.op('pe', lambda e, dst=dst, Rb=Rb, q=q, sub=sub: e.matmul(dst, lhsT=sel2, rhs=Rb[:, q, sub * 256:(sub + 1) * 256], start=True, stop=True),
                         reads=[rk_, 'rw_cst'], writes=[pk])
                Wb, NKb, Bb, KDb, Rr = views
                for u in range(4):
                    t = t0 + sub * 4 + u
                    o = u * 64
                    S.op('dve', lambda e, NKb=NKb, o=o: e.scalar_tensor_tensor(out=junk[:], in0=St[:], scalar=1.0, in1=NKb[:, o:o + 64], op0=ALU.mult, op1=ALU.mult, accum_out=sa[:]),
                         reads=[pk, 'rwS'], writes=['rwsa', 'rwjunk'])
                    S.op('dve', lambda e, Wb=Wb, o=o: e.tensor_tensor(out=St[:], in0=St[:], in1=Wb[:, o:o + 64], op=ALU.mult), reads=[pk, 'rwS'], writes=['rwS'])
                    S.op('dve', lambda e, Bb=Bb, o=o: e.scalar_tensor_tensor(out=St[:], in0=Bb[:, o:o + 64], scalar=sa[:, 0:1], in1=St[:], op0=ALU.mult, op1=ALU.add),
                         reads=[pk, 'rwS', 'rwsa'], writes=['rwS'])
                    S.op('dve', lambda e, KDb=KDb, o=o, t=t: e.scalar_tensor_tensor(out=St[:], in0=KDb[:, o:o + 64], scalar=V[:, t:t + 1], in1=St[:], op0=ALU.mult, op1=ALU.add),
                         reads=[pk, 'rwS', 'rw_V'], writes=['rwS'])
                    S.op('dve', lambda e, Rr=Rr, o=o, t=t: e.scalar_tensor_tensor(out=junk2[:], in0=St[:], scalar=1.0, in1=Rr[:, o:o + 64], op0=ALU.mult, op1=ALU.mult, accum_out=Y[:, t:t + 1]),
                         reads=[pk, 'rwS'], writes=['rw_Y', 'rwjunk2'])

    if 'rwY' in dr:
        S.dma(lambda e: e.dma_start(out=dr['rwY'][:, :], in_=Y[:, :]), reads=['rw_Y'])
        S.barrier()
    with Scope_(nc, S) as sb4:
        T1 = [sb4(f"rwT1{i}", [128, 64]) for i in range(2)]
        ys = [sb4(f"rwys{i}", [64, 128]) for i in range(2)]
        cen = [sb4(f"rwcen{i}", [64, 128]) for i in range(2)]
        sq = [sb4(f"rwsq{i}", [64, 128]) for i in range(2)]
        rs_ = [sb4(f"rwrs{i}", [64, 128]) for i in range(2)]
        bg = [sb4(f"rwbg{i}", [64, 2, 128]) for i in range(2)]
        epsb = sb4("rweps", [64, 1])
        S.op('dve', lambda e: e.memset(epsb[:], 64e-5), writes=['rweps'])
        for vt in range(NTILE):
            a = vt % 2
            sb1 = (1 - vt) if vt < 2 else 131 - vt
            s0 = vt * 128
            S.dma(lambda e, a=a, s0=s0: e.dma_start(out=bg[a][:], in_=BG[:, :, s0:s0 + 128].rearrange("m c t -> c m t")), writes=[f'rwbg{a}'])
            S.op('pe', lambda e, sb1=sb1, a=a: e.matmul(P[a][:, 0:64], lhsT=Y[:, sb1 * 128:(sb1 + 1) * 128], rhs=idj[:, 0, 64:128], start=True, stop=True),
                 reads=['rw_Y', 'rw_idj'], writes=[f'P{a}'])
            S.op('act', lambda e, a=a: e.copy(out=T1[a][:], in_=P[a][:, 0:64]), reads=[f'P{a}'], writes=[f'rwT1{a}'])
            S.op('pe', lambda e, a=a: e.matmul(P[2 + a][0:64, 0:128], lhsT=T1[a][:], rhs=idj[:, 1, :], start=True, stop=True),
                 reads=[f'rwT1{a}', 'rw_idj'], writes=[f'P{2+a}'])
            S.op('dve', lambda e, a=a, s0=s0: e.tensor_tensor(out=ys[a][:], in0=P[2 + a][0:64, 0:128], in1=Y[0:64, s0:s0 + 128], op=ALU.add),
                 reads=[f'P{2+a}', 'rw_Y'], writes=[f'rwys{a}'])
            S.op('pe', lambda e, a=a: e.matmul(P[4 + a][0:64, 0:128], lhsT=cst[0:64, 0, 0:64], rhs=ys[a][:], start=True, stop=True),
                 reads=[f'rwys{a}', 'rw_cst'], writes=[f'P{4+a}'])
            S.op('dve', lambda e, a=a: e.scalar_tensor_tensor(out=cen[a][:], in0=P[4 + a][0:64, 0:128], scalar=-1.0 / 64, in1=ys[a][:], op0=ALU.mult, op1=ALU.add),
                 reads=[f'P{4+a}', f'rwys{a}'], writes=[f'rwcen{a}'])
            S.op('pool', lambda e, a=a: e.tensor_tensor(out=sq[a][:], in0=cen[a][:], in1=cen[a][:], op=ALU.mult), reads=[f'rwcen{a}'], writes=[f'rwsq{a}'])
            S.op('pe', lambda e, a=a: e.matmul(P[6 + a][0:64, 0:128], lhsT=cst[0:64, 0, 0:64], rhs=sq[a][:], start=True, stop=True),
                 reads=[f'rwsq{a}', 'rw_cst'], writes=[f'P{6+a}'])
            S.op('act', lambda e, a=a: e.activation(out=rs_[a][:], in_=P[6 + a][0:64, 0:128], func=AF.Sqrt, bias=epsb[:, 0:1], scale=1.0 / 64),
                 reads=[f'P{6+a}', 'rweps'], writes=[f'rwrs{a}'])
            S.op('dve', lambda e, a=a: e.reciprocal(out=rs_[a][:], in_=rs_[a][:]), reads=[f'rwrs{a}'], writes=[f'rwrs{a}'])
            S.op('dve', lambda e, a=a: e.tensor_tensor(out=cen[a][:], in0=cen[a][:], in1=rs_[a][:], op=ALU.mult), reads=[f'rwcen{a}', f'rwrs{a}'], writes=[f'rwcen{a}'])
            S.op('dve', lambda e, a=a: e.tensor_scalar(out=cen[a][:], in0=cen[a][:], scalar1=par[0:64, 17:18], scalar2=par[0:64, 18:19], op0=ALU.mult, op1=ALU.add),
                 reads=[f'rwcen{a}', 'rw_par'], writes=[f'rwcen{a}'])
            S.op('dve', lambda e, a=a: e.tensor_tensor(out=cen[a][:], in0=cen[a][:], in1=bg[a][:, 0, :], op=ALU.add), reads=[f'rwcen{a}', f'rwbg{a}'], writes=[f'rwcen{a}'])
            S.op('dve', lambda e, a=a: e.tensor_tensor(out=sq[a][:], in0=cen[a][:], in1=bg[a][:, 1, :], op=ALU.mult), reads=[f'rwcen{a}', f'rwbg{a}'], writes=[f'rwsq{a}'])
            for (k, m0, n, c0) in ytile_dst(vt):
                S.dma(lambda e, a=a, k=k, m0=m0, n=n, c0=c0: e.dma_start(out=yT[k, 128:192, m0:m0 + n], in_=sq[a][:, c0:c0 + n]), reads=[f'rwsq{a}'])


ML0 = 768

def ml_params(inp, l, j):
    cw, cb = inp['ml_conv_w'][l], inp['ml_conv_b'][l]
    ch = np.arange(64)
    cols = np.concatenate([64 * j + ch, 256 + 64 * j + ch])
    par = np.zeros((128, 12), np.float32)
    par[:, 0] = cw[0, cols]; par[:, 1] = cw[1, cols]; par[:, 2] = cw[2, cols]; par[:, 3] = cb[cols]
    par[:, 4] = cw[2, cols]; par[:, 5] = cw[1, cols]; par[:, 6] = cw[0, cols]; par[:, 7] = cb[cols]
    for v in range(2):
        par[:, 8 + v] = inp['ml_i_bias'][l][v, j]
        par[:, 10 + v] = inp['ml_f_bias'][l][v, j]
    nw = np.repeat(inp['ml_norm_w'][l][None, 64 * j:64 * j + 64], 128, 0).astype(np.float32)
    return par, np.ascontiguousarray(nw)


def ml_consts():
    c = np.zeros((3, 128, 128), np.float32)
    c[0] = np.triu(np.ones((128, 128), np.float32))
    for base in (0, 64):
        for b32 in (0, 32):
            for c_ in range(16):
                dst = base + b32 + c_
                c[1, dst + 16, dst] = -1.0
                c[1, dst, dst + 16] = 1.0
    c[2] = 1.0
    return c


def rope_tables():
    inv = (np.float32(10000.0) ** (-np.arange(16, dtype=np.float32) / np.float32(16))).astype(np.float32)
    pos = np.arange(16384)
    comp = np.stack([pos // 64, pos % 64]).astype(np.float32)
    tab = np.zeros((2, 2, 64, 16384), np.float32)
    for ch in range(64):
        half, f = ch // 32, ch % 16
        ang = (comp[half] * inv[f]).astype(np.float32)
        tab[0, 0, ch] = np.cos(ang); tab[0, 1, ch] = np.sin(ang)
    tab[1] = tab[0][:, :, ::-1]
    return np.ascontiguousarray(tab)


def stage_ml(nc, S, sb, P, dr, Scope_):
    pT, pN, yT, QK = dr['pT'], dr['pN'], dr['yT'], dr['mlQK']
    par = sb("ml_par_sb", [128, 12])
    cst = sb("ml_cst_sb", [128, 3, 128])
    idj = sb("ml_idj", [128, 2, 128])
    nwb = sb("ml_nw_sb", [128, 64])
    S.dma(lambda e: e.dma_start(out=par[:], in_=dr['ml_par'][:, :]), writes=['ml_par'])
    S.dma(lambda e: e.dma_start(out=cst[:], in_=dr['ml_cst'].rearrange("m p n -> p m n")), writes=['ml_cst'])
    S.dma(lambda e: e.dma_start(out=idj[:], in_=dr['ident2'].rearrange("j p n -> p j n")), writes=['ml_idj'])
    S.dma(lambda e: e.dma_start(out=nwb[:], in_=dr['ml_nw'][:, :]), writes=['ml_nw'])
    tri = cst[:, 0, :]
    rot = cst[:, 1, :]
    allones = cst[:, 2, :]
    E = [sb(f"ml_E{v}", [128, NTILE]) for v in range(2)]
    CL = [sb(f"ml_CL{v}", [128, NTILE]) for v in range(2)]
    GM = [sb(f"ml_GM{v}", [128, NTILE]) for v in range(2)]
    one1 = sb("ml_one1", [128, 1])
    S.op('dve', lambda e: e.memset(one1[:], 1.0), writes=['ml_one1'])

    with Scope_(nc, S) as sb2:
        X = [sb2(f"mlX{i}", [128, 514]) for i in range(2)]
        cs = [sb2(f"mlcs{i}", [128, 2, 512]) for i in range(2)]
        t1 = [sb2(f"mlt1{i}", [128, 512]) for i in range(2)]
        uu = [sb2(f"mluu{i}", [128, 512]) for i in range(2)]
        t2 = [sb2(f"mlt2{i}", [128, 512]) for i in range(2)]
        n = 0
        for view in range(2):
            pc = 4 * view
            for blk in range(33):
                a = n % 2; n += 1
                s0 = 0 if blk == 0 else 256 + (blk - 1) * 512
                NB = 256 if blk == 0 else 512
                lz = blk in (0, 1); rz = blk in (0, 32)
                lo = s0 - (0 if lz else 1); hi = s0 + NB + (0 if rz else 1); c_lo = 1 if lz else 0
                Xa = X[a]; xk = f'mlX{a}'
                if lz:
                    S.op('pool', lambda e, Xa=Xa: e.memset(Xa[:, 0:1], 0.0), writes=[xk])
                if rz:
                    S.op('pool', lambda e, Xa=Xa, NB=NB: e.memset(Xa[:, NB + 1:NB + 2], 0.0), writes=[xk])
                S.dma(lambda e, Xa=Xa, view=view, lo=lo, hi=hi, c_lo=c_lo: e.dma_start(out=Xa[:, c_lo:c_lo + hi - lo], in_=pT[view, ML0:ML0 + 128, lo:hi]), writes=[xk])
                if blk > 0:
                    p0 = s0 - 256
                    for hh in range(2):
                        S.dma(lambda e, a=a, view=view, p0=p0, hh=hh: e.dma_start(
                            out=cs[a][hh * 64:(hh + 1) * 64, :, :], in_=dr['rope'][view, :, :, p0:p0 + 512].rearrange("m c t -> c m t")), writes=[f'mlcs{a}'])
                ta = t1[a]; tk = f'mlt1{a}'
                S.op('dve', lambda e, Xa=Xa, ta=ta, NB=NB, pc=pc: e.tensor_scalar(out=ta[:, 0:NB], in0=Xa[:, 1:NB + 1], scalar1=par[:, pc + 1:pc + 2], scalar2=par[:, pc + 3:pc + 4], op0=ALU.mult, op1=ALU.add),
                     reads=[xk, 'ml_par'], writes=[tk])
                S.op('dve', lambda e, Xa=Xa, ta=ta, NB=NB, pc=pc: e.scalar_tensor_tensor(out=ta[:, 0:NB], in0=Xa[:, 0:NB], scalar=par[:, pc:pc + 1], in1=ta[:, 0:NB], op0=ALU.mult, op1=ALU.add),
                     reads=[xk, tk, 'ml_par'], writes=[tk])
                S.op('dve', lambda e, Xa=Xa, ta=ta, NB=NB, pc=pc: e.scalar_tensor_tensor(out=ta[:, 0:NB], in0=Xa[:, 2:NB + 2], scalar=par[:, pc + 2:pc + 3], in1=ta[:, 0:NB], op0=ALU.mult, op1=ALU.add),
                     reads=[xk, tk, 'ml_par'], writes=[tk])
                ua = uu[a]; uk = f'mluu{a}'
                S.op('act', lambda e, ta=ta, ua=ua, NB=NB: e.activation(out=ua[:, 0:NB], in_=ta[:, 0:NB], func=AF.Silu), reads=[tk], writes=[uk])
                if blk > 0:
                    S.op('pe', lambda e, ua=ua, a=a: e.matmul(P[a][:, 0:512], lhsT=rot, rhs=ua[:, 0:512], start=True, stop=True), reads=[uk, 'ml_cst'], writes=[f'P{a}'])
                    tb = t2[a]; bk = f'mlt2{a}'
                    S.op('dve', lambda e, tb=tb, a=a: e.tensor_tensor(out=tb[:, :], in0=P[a][:, 0:512], in1=cs[a][:, 1, :], op=ALU.mult), reads=[f'P{a}', f'mlcs{a}'], writes=[bk])
                    S.op('pool', lambda e, ua=ua, a=a: e.tensor_tensor(out=ua[:, :], in0=ua[:, :], in1=cs[a][:, 0, :], op=ALU.mult), reads=[uk, f'mlcs{a}'], writes=[uk])
                    S.op('dve', lambda e, ua=ua, tb=tb: e.tensor_tensor(out=ua[:, :], in0=ua[:, :], in1=tb[:, :], op=ALU.add), reads=[uk, bk], writes=[uk])
                S.op('dve', lambda e, ua=ua, NB=NB: e.tensor_scalar(out=ua[64:128, 0:NB], in0=ua[64:128, 0:NB], scalar1=0.125, scalar2=None, op0=ALU.mult), reads=[uk], writes=[uk])
                S.dma(lambda e, ua=ua, view=view, s0=s0, NB=NB: e.dma_start(out=QK[view, :, s0:s0 + NB], in_=ua[:, 0:NB]), reads=[uk])

        gr = [sb2(f"mlgr{i}", [128, 128]) for i in range(2)]
        G = {}
        for view in range(2):
            for gi_, row in ((0, 1024 + view), (1, 1026 + view)):
                Gt = sb2(f"mlG{view}{gi_}", [128, NTILE])
                G[(view, gi_)] = Gt
                gk = f'mlG{view}{gi_}'
                src = pT[view, ML0 + row - 768, :]
                a = gi_
                S.dma(lambda e, a=a, src=src: e.dma_start(out=gr[a][:, :], in_=src[0:128 * 128].rearrange("(c p) -> c p", p=128)), writes=[f'mlgr{a}'])
                S.op('pe', lambda e, a=a: e.transpose(P[2 + a][:, 0:128], gr[a][:, :], idj[:, 0, :]), reads=[f'mlgr{a}', 'ml_idj'], writes=[f'P{2+a}'])
                S.op('dve', lambda e, a=a, Gt=Gt: e.tensor_copy(out=Gt[:, 0:128], in_=P[2 + a][:, 0:128]), reads=[f'P{2+a}'], writes=[gk])
                S.dma(lambda e, a=a, src=src: e.dma_start(out=gr[a][0:2, :], in_=src[128 * 128:130 * 128].rearrange("(c p) -> c p", p=128)), writes=[f'mlgr{a}'])
                S.op('pe', lambda e, a=a: e.transpose(P[2 + a][:, 0:2], gr[a][0:2, :], idj[0:2, 0, 0:2]), reads=[f'mlgr{a}', 'ml_idj'], writes=[f'P{2+a}'])
                S.op('dve', lambda e, a=a, Gt=Gt: e.tensor_copy(out=Gt[:, 128:130], in_=P[2 + a][:, 0:2]), reads=[f'P{2+a}'], writes=[gk])
        w = [sb2(f"mlw{i}", [128, NTILE]) for i in range(6)]
        mrow = sb2("mlmrow", [1, NTILE])
        mcol = sb2("mlmcol", [128, 2])
        nfb = sb2("mlnfb", [128, 2])
        S.op('dve', lambda e: e.tensor_scalar(out=nfb[:], in0=par[:, 10:12], scalar1=-1.0, scalar2=None, op0=ALU.mult), reads=['ml_par'], writes=['mlnfb'], hz=True)
        for view in range(2):
            Gi, Gf = G[(view, 0)], G[(view, 1)]
            lf, B, tot, u, ut, mb = w
            S.op('act', lambda e, Gf=Gf, view=view: e.activation(out=lf[:], in_=Gf[:], func=AF.Exp, bias=nfb[:, view:view + 1], scale=-1.0), reads=[f'mlG{view}1', 'mlnfb'], writes=['mlw0'])
            S.op('act', lambda e: e.activation(out=lf[:], in_=lf[:], func=AF.Ln, bias=one1[:, 0:1], scale=1.0), reads=['mlw0', 'ml_one1'], writes=['mlw0'])
            S.op('dve', lambda e: e.tensor_scalar(out=lf[:], in0=lf[:], scalar1=-1.0, scalar2=None, op0=ALU.mult), reads=['mlw0'], writes=['mlw0'])
            S.op('pe', lambda e: e.matmul(P[0][:, 0:NTILE], lhsT=tri, rhs=lf[:], start=True, stop=True), reads=['mlw0', 'ml_cst'], writes=['P0'])
            S.op('pe', lambda e: e.matmul(P[1][:, 0:NTILE], lhsT=allones, rhs=lf[:], start=True, stop=True), reads=['mlw0', 'ml_cst'], writes=['P1'])
            S.op('dve', lambda e: e.tensor_copy(out=tot[:], in_=P[1][:, 0:NTILE]), reads=['P1'], writes=['mlw2'])
            S.op('dve', lambda e: e.tensor_tensor_scan(out=B[:], data0=allones[:, 0:1].broadcast_to([128, NTILE]), data1=tot[:], initial=0.0, op0=ALU.mult, op1=ALU.add),
                 reads=['mlw2', 'ml_cst'], writes=['mlw1'])
            S.op('dve', lambda e: e.tensor_tensor(out=B[:], in0=B[:], in1=tot[:], op=ALU.subtract), reads=['mlw1', 'mlw2'], writes=['mlw1'])
            S.op('dve', lambda e: e.tensor_tensor(out=B[:], in0=B[:], in1=P[0][:, 0:NTILE], op=ALU.add), reads=['mlw1', 'P0'], writes=['mlw1'])
            S.op('dve', lambda e, Gi=Gi, view=view: e.scalar_tensor_tensor(out=u[:], in0=Gi[:], scalar=par[:, 8 + view:9 + view], in1=B[:], op0=ALU.add, op1=ALU.subtract),
                 reads=[f'mlG{view}0', 'ml_par', 'mlw1'], writes=['mlw3'])
            S.op('pe', lambda e: e.transpose(P[2][:, 0:128], u[:, 0:128], idj[:, 0, :]), reads=['mlw3', 'ml_idj'], writes=['P2'])
            S.op('pe', lambda e: e.transpose(P[3][0:2, 0:128], u[:, 128:130], idj[:, 0, :]), reads=['mlw3', 'ml_idj'], writes=['P3'])
            S.op('dve', lambda e: e.tensor_reduce(out=mcol[:, 0:1], in_=P[2][:, 0:128], axis=mybir.AxisListType.X, op=ALU.max), reads=['P2'], writes=['mlmcol'], hz=True)
            S.op('dve', lambda e: e.tensor_reduce(out=mcol[0:2, 1:2], in_=P[3][0:2, 0:128], axis=mybir.AxisListType.X, op=ALU.max), reads=['P3'], writes=['mlmcol'], hz=True)
            S.op('pe', lambda e: e.transpose(P[2][0:1, 0:128], mcol[:, 0:1], idj[:, 0, :]), reads=['mlmcol', 'ml_idj'], writes=['P2'])
            S.op('pe', lambda e: e.transpose(P[3][0:1, 0:2], mcol[0:2, 1:2], idj[0:2, 0, 0:2]), reads=['mlmcol', 'ml_idj'], writes=['P3'])
            S.op('dve', lambda e: e.tensor_copy(out=mrow[:, 0:128], in_=P[2][0:1, 0:128]), reads=['P2'], writes=['mlmrow'], hz=True)
            S.op('dve', lambda e: e.tensor_copy(out=mrow[:, 128:130], in_=P[3][0:1, 0:2]), reads=['P3'], writes=['mlmrow'], hz=True)
            S.op('dve', lambda e: e.tensor_tensor_scan(out=mrow[:], data0=allones[0:1, 0:1].broadcast_to([1, NTILE]), data1=mrow[:], initial=0.0, op0=ALU.mult, op1=ALU.max),
                 reads=['mlmrow', 'ml_cst'], writes=['mlmrow'])
            S.op('pe', lambda e: e.matmul(P[0][:, 0:NTILE], lhsT=allones[0:1, :], rhs=mrow[:], start=True, stop=True), reads=['mlmrow', 'ml_cst'], writes=['P0'])
            S.op('dve', lambda e: e.tensor_copy(out=mb[:], in_=P[0][:, 0:NTILE]), reads=['P0'], writes=['mlw5'])
            S.op('dve', lambda e: e.tensor_tensor(out=u[:], in0=u[:], in1=mb[:], op=ALU.subtract), reads=['mlw3', 'mlw5'], writes=['mlw3'])
            S.op('act', lambda e, view=view: e.activation(out=E[view][:], in_=u[:], func=AF.Exp), reads=['mlw3'], writes=[f'ml_E{view}'])
            S.op('dve', lambda e: e.tensor_tensor(out=B[:], in0=B[:], in1=mb[:], op=ALU.add), reads=['mlw1', 'mlw5'], writes=['mlw1'])
            S.op('act', lambda e, view=view: e.activation(out=CL[view][:], in_=B[:], func=AF.Exp, scale=-1.0), reads=['mlw1'], writes=[f'ml_CL{view}'])
            S.op('dve', lambda e: e.tensor_tensor(out=ut[:, 1:NTILE], in0=mb[:, 0:NTILE - 1], in1=mb[:, 1:NTILE], op=ALU.subtract), reads=['mlw5'], writes=['mlw4'], hz=True)
            S.op('dve', lambda e: e.tensor_scalar(out=ut[:, 0:1], in0=mb[:, 0:1], scalar1=-1.0, scalar2=None, op0=ALU.mult), reads=['mlw5'], writes=['mlw4'], hz=True)
            S.op('act', lambda e, view=view: e.activation(out=GM[view][:], in_=ut[:], func=AF.Exp), reads=['mlw4'], writes=[f'ml_GM{view}'])

    with Scope_(nc, S) as sb3:
        H1 = sb3("mlH1", [128, NTILE, 64])
        Qc = [sb3(f"mlQc{i}", [64, 128]) for i in range(2)]
        Kc = [sb3(f"mlKc{i}", [64, 128]) for i in range(2)]
        Vc = [sb3(f"mlVc{i}", [128, 65]) for i in range(2)]
        Oc = [sb3(f"mlOc{i}", [128, 64]) for i in range(2)]
        Kh = [sb3(f"mlKh{i}", [128, 64]) for i in range(2)]
        Am = [sb3(f"mlAm{i}", [128, 128]) for i in range(2)]
        Cg = [sb3(f"mlCg{i}", [64, 65]) for i in range(2)]
        C = sb3("mlC", [64, 65])
        den = [sb3(f"mlden{i}", [128, 1]) for i in range(2)]
        den2 = [sb3(f"mlden2{i}", [128, 1]) for i in range(2)]
        hh_ = [sb3(f"mlh{i}", [128, 64]) for i in range(2)]
        st6 = [sb3(f"mlst{i}", [128, 6]) for i in range(2)]
        mv = [sb3(f"mlmv{i}", [128, 2]) for i in range(2)]
        yo = [sb3(f"mlyo{i}", [64, 128]) for i in range(2)]
        epsb = sb3("mleps", [128, 1])
        S.op('dve', lambda e: e.memset(epsb[:], 1e-6), writes=['mleps'])
        for i in range(2):
            S.op('dve', lambda e, i=i: e.memset(Vc[i][:, 64:65], 1.0), writes=[f'mlVc{i}'])
        for view in (1, 0):
            S.op('dve', lambda e: e.memset(C[:], 0.0), writes=['mlC'])
            for c in range(NTILE):
                a = c % 2
                s0 = c * 128
                S.dma(lambda e, a=a, view=view, s0=s0: e.dma_start(out=Qc[a][:], in_=QK[view, 0:64, s0:s0 + 128]), writes=[f'mlQc{a}'])
                S.dma(lambda e, a=a, view=view, s0=s0: e.dma_start(out=Kc[a][:], in_=QK[view, 64:128, s0:s0 + 128]), writes=[f'mlKc{a}'])
                S.dma(lambda e, a=a, view=view, s0=s0: e.dma_start(out=Vc[a][:, 0:64], in_=pN[view, s0:s0 + 128, 128:192]), writes=[f'mlVc{a}'])
                ecol = E[view][:, c:c + 1]
                S.op('pe', lambda e, a=a: e.transpose(P[a][:, 0:64], Kc[a][:], idj[0:64, 0, 0:64]), reads=[f'mlKc{a}', 'ml_idj'], writes=[f'P{a}'])
                S.op('dve', lambda e, a=a, ecol=ecol: e.tensor_scalar(out=Kh[a][:], in0=P[a][:, 0:64], scalar1=ecol, scalar2=None, op0=ALU.mult),
                     reads=[f'P{a}', f'ml_E{view}'], writes=[f'mlKh{a}'])
                S.op('pe', lambda e, a=a: e.matmul(P[2 + a][:, 0:128], lhsT=Kc[a][:], rhs=Qc[a][:], start=True, stop=True), reads=[f'mlKc{a}', f'mlQc{a}'], writes=[f'P{2+a}'])
                S.op('dve', lambda e, a=a, ecol=ecol: e.scalar_tensor_tensor(out=Am[a][:], in0=P[2 + a][:, 0:128], scalar=ecol, in1=tri, op0=ALU.mult, op1=ALU.mult),
                     reads=[f'P{2+a}', f'ml_E{view}', 'ml_cst'], writes=[f'mlAm{a}'])
                S.op('dve', lambda e, a=a, view=view, c=c: e.tensor_scalar(out=Cg[a][:], in0=C[:], scalar1=GM[view][0:64, c:c + 1], scalar2=None, op0=ALU.mult),
                     reads=['mlC', f'ml_GM{view}'], writes=[f'mlCg{a}'])
                S.op('pe', lambda e, a=a: e.matmul(P[4 + a][:, 0:65], lhsT=Am[a][:], rhs=Vc[a][:], start=True, stop=False), reads=[f'mlAm{a}', f'mlVc{a}'], writes=[f'P{4+a}'])
                S.op('pe', lambda e, a=a: e.matmul(P[4 + a][:, 0:65], lhsT=Qc[a][:], rhs=Cg[a][:], start=False, stop=True), reads=[f'mlQc{a}', f'mlCg{a}'], writes=[f'P{4+a}'])
                S.op('pe', lambda e, a=a: e.matmul(P[6 + a][0:64, 0:65], lhsT=Kh[a][:], rhs=Vc[a][:], start=True, stop=True), reads=[f'mlKh{a}', f'mlVc{a}'], writes=[f'P{6+a}'])
                S.op('dve', lambda e, a=a: e.tensor_tensor(out=C[:], in0=Cg[a][:], in1=P[6 + a][0:64, 0:65], op=ALU.add), reads=[f'mlCg{a}', f'P{6+a}'], writes=['mlC'])
                S.op('dve', lambda e, a=a: e.tensor_scalar(out=den2[a][:], in0=P[4 + a][:, 64:65], scalar1=-1.0, scalar2=None, op0=ALU.mult),
                     reads=[f'P{4+a}'], writes=[f'mlden2{a}'], hz=True)
                S.op('dve', lambda e, a=a, view=view, c=c: e.scalar_tensor_tensor(out=den[a][:], in0=P[4 + a][:, 64:65], scalar=CL[view][:, c:c + 1], in1=den2[a][:], op0=ALU.max, op1=ALU.max),
                     reads=[f'P{4+a}', f'ml_CL{view}', f'mlden2{a}'], writes=[f'mlden{a}'], hz=True)
                S.op('dve', lambda e, a=a: e.reciprocal(out=den[a][:], in_=den[a][:]), reads=[f'mlden{a}'], writes=[f'mlden{a}'], hz=True)
                S.op('dve', lambda e, a=a: e.tensor_scalar(out=hh_[a][:], in0=P[4 + a][:, 0:64], scalar1=den[a][:, 0:1], scalar2=None, op0=ALU.mult),
                     reads=[f'P{4+a}', f'mlden{a}'], writes=[f'mlh{a}'])
                if view == 1:
                    vt = (1 - c) if c < 2 else 131 - c
                    S.op('pe', lambda e, a=a: e.matmul(P[a][:, 64:128], lhsT=idj[:, 1, :], rhs=hh_[a][:], start=True, stop=True), reads=[f'mlh{a}', 'ml_idj'], writes=[f'P{a}'])
                    S.op('act', lambda e, a=a, vt=vt: e.copy(out=H1[:, vt, :], in_=P[a][:, 64:128]), reads=[f'P{a}'], writes=['mlH1'])
                else:
                    S.dma(lambda e, a=a, s0=s0: e.dma_start(out=Oc[a][:], in_=pN[0, s0:s0 + 128, 192:256]), writes=[f'mlOc{a}'])
                    S.op('dve', lambda e, a=a, c=c: e.tensor_tensor(out=hh_[a][:], in0=hh_[a][:], in1=H1[:, c, :], op=ALU.add), reads=[f'mlh{a}', 'mlH1'], writes=[f'mlh{a}'])
                    S.op('dve', lambda e, a=a: e.bn_stats(out=st6[a][:], in_=hh_[a][:]), reads=[f'mlh{a}'], writes=[f'mlst{a}'], hz=True)
                    S.op('dve', lambda e, a=a: e.bn_aggr(out=mv[a][:], in_=st6[a][:]), reads=[f'mlst{a}'], writes=[f'mlmv{a}'], hz=True)
                    S.op('act', lambda e, a=a: e.activation(out=mv[a][:, 1:2], in_=mv[a][:, 1:2], func=AF.Sqrt, bias=epsb[:, 0:1], scale=1.0), reads=[f'mlmv{a}', 'mleps'], writes=[f'mlmv{a}'])
                    S.op('dve', lambda e, a=a: e.reciprocal(out=mv[a][:, 1:2], in_=mv[a][:, 1:2]), reads=[f'mlmv{a}'], writes=[f'mlmv{a}'], hz=True)
                    S.op('dve', lambda e, a=a: e.tensor_scalar(out=hh_[a][:], in0=hh_[a][:], scalar1=mv[a][:, 0:1], scalar2=mv[a][:, 1:2], op0=ALU.subtract, op1=ALU.mult),
                         reads=[f'mlh{a}', f'mlmv{a}'], writes=[f'mlh{a}'])
                    S.op('dve', lambda e, a=a: e.tensor_tensor(out=hh_[a][:], in0=hh_[a][:], in1=nwb[:], op=ALU.mult), reads=[f'mlh{a}', 'ml_nw'], writes=[f'mlh{a}'])
                    S.op('act', lambda e, a=a: e.activation(out=Oc[a][:], in_=Oc[a][:], func=AF.Sigmoid), reads=[f'mlOc{a}'], writes=[f'mlOc{a}'])
                    S.op('dve', lambda e, a=a: e.tensor_tensor(out=hh_[a][:], in0=hh_[a][:], in1=Oc[a][:], op=ALU.mult), reads=[f'mlh{a}', f'mlOc{a}'], writes=[f'mlh{a}'])
                    S.op('pe', lambda e, a=a: e.transpose(P[a][0:64, 128:256], hh_[a][:], idj[:, 0, :]), reads=[f'mlh{a}', 'ml_idj'], writes=[f'P{a}'])
                    S.op('act', lambda e, a=a: e.copy(out=yo[a][:], in_=P[a][0:64, 128:256]), reads=[f'P{a}'], writes=[f'mlyo{a}'])
                    for (k, m0, nn, c0) in ytile_dst(c):
                        S.dma(lambda e, a=a, k=k, m0=m0, nn=nn, c0=c0: e.dma_start(out=yT[k, 192:256, m0:m0 + nn], in_=yo[a][:, c0:c0 + nn]), reads=[f'mlyo{a}'])


S3_TILES = [(0, 64)] + [(64 + 128 * i, 128) for i in range(32)]
S3_GROUPS = [list(range(0, 11)), list(range(11, 22)), list(range(22, 33))]

def w_out_perm_rows():
    rows = []
    for r in range(4):
        rows += list(128 * r + np.arange(128)) + list(512 + 64 * r + np.arange(64)) + list(768 + 64 * r + np.arange(64))
    return np.array(rows)


def stage3(nc, S, sb, P, dr, Scope_, last):
    yG, x_sh, modT_d, x1_d, H2T_d, x_out = dr.get('yG'), dr['x_sh'], dr['modT_out'], dr['x1_scr'], dr['H2T_scr'], dr['x_out']
    idj = sb("s3_idj", [128, 2, 128])
    ones = sb("s3_ones", [128, 128])
    WT = sb("s3_WT", [128, 33, 32])
    eps5 = sb("s3_eps", [128, 1])
    S.dma(lambda e: e.dma_start(out=idj[:], in_=dr['ident2'].rearrange("j p n -> p j n")), writes=['s3_idj'])
    S.op('dve', lambda e: e.memset(ones[:], 1.0), writes=['s3_ones'])
    S.op('dve', lambda e: e.memset(eps5[:], 1e-5), writes=['s3_eps'])

    def layer_norm_tile(S, z, zk, n, st6, mv, lng, lnb, lnkey, out, outk, tagk):
        for h in range(2):
            S.op('dve', lambda e, h=h: e.bn_stats(out=st6[0:n, h, :], in_=z[0:n, h * 512:(h + 1) * 512]), reads=[zk], writes=[tagk + 'st'], hz=True)
        S.op('dve', lambda e: e.bn_aggr(out=mv[0:n, :], in_=st6[0:n, :, :].rearrange("p a b -> p (a b)")), reads=[tagk + 'st'], writes=[tagk + 'mv'], hz=True)
        S.op('act', lambda e: e.activation(out=mv[0:n, 1:2], in_=mv[0:n, 1:2], func=AF.Sqrt, bias=eps5[0:n, 0:1], scale=1.0), reads=[tagk + 'mv', 's3_eps'], writes=[tagk + 'mv'])
        S.op('dve', lambda e: e.reciprocal(out=mv[0:n, 1:2], in_=mv[0:n, 1:2]), reads=[tagk + 'mv'], writes=[tagk + 'mv'], hz=True)
        S.op('dve', lambda e: e.tensor_scalar(out=out[0:n, :], in0=z[0:n, :], scalar1=mv[0:n, 0:1], scalar2=mv[0:n, 1:2], op0=ALU.subtract, op1=ALU.mult),
             reads=[zk, tagk + 'mv'], writes=[outk])
        S.op('pool', lambda e: e.tensor_tensor(out=out[0:n, :], in0=out[0:n, :], in1=lng[0:n, :], op=ALU.mult), reads=[outk, lnkey], writes=[outk])
        S.op('dve', lambda e: e.tensor_tensor(out=out[0:n, :], in0=out[0:n, :], in1=lnb[0:n, :], op=ALU.add), reads=[outk, lnkey], writes=[outk])

    with Scope_(nc, S) as sb2:
        Wo = sb2("s3_Wo", [128, 8, 1024])
        Wr = sb2("s3_Wr", [128, 8, 36])
        rb = sb2("s3_rb", [128, 36])
        mT = sb2("s3_mT", [128, 48, 2])
        bc = sb2("s3_bc", [128, 2, 3, 1024])
        ln = sb2("s3_ln", [128, 2, 1024])
        Dg = [sb2(f"s3_D{i}", [128, 128]) for i in range(2)]
        S.dma(lambda e: e.dma_start(out=Wo[:], in_=dr['w_out_p'].rearrange("(kc p) n -> p kc n", p=128)), writes=['s3_Wo'])
        S.dma(lambda e: e.dma_start(out=Wr[:], in_=dr['w_r'].rearrange("(kc p) n -> p kc n", p=128)), writes=['s3_Wr'])
        S.dma(lambda e: e.dma_start(out=rb[:], in_=dr['r_b'][:, :]), writes=['s3_rb'])
        S.dma(lambda e: e.dma_start(out=mT[:], in_=modT_d.rearrange("p (c j) -> p c j", j=2)), writes=['s3_mT'])
        S.dma(lambda e: e.dma_start(out=ln[:], in_=dr['ln_bc'][0:2].rearrange("m p n -> p m n")), writes=['s3_ln'])
        di = 0
        for j in range(2):
            for vi, vec in enumerate((2, 4, 3)):
                for c8 in range(8):
                    col = vec * 8 + c8
                    Dt = Dg[di % 2]; dk = f's3_D{di%2}'; pslot = di % 2; di += 1
                    S.op('dve', lambda e, Dt=Dt, col=col, j=j: e.tensor_scalar(out=Dt[:], in0=idj[:, 0, :], scalar1=mT[:, col, j:j + 1], scalar2=None, op0=ALU.mult),
                         reads=['s3_idj', 's3_mT'], writes=[dk])
                    S.op('pe', lambda e, Dt=Dt, pslot=pslot: e.matmul(P[pslot][:, 0:128], lhsT=ones[:], rhs=Dt[:], start=True, stop=True), reads=[dk, 's3_ones'], writes=[f'P{pslot}'])
                    if vec == 4:
                        S.op('act', lambda e, pslot=pslot, j=j, vi=vi, c8=c8: e.activation(out=bc[:, j, vi, c8 * 128:(c8 + 1) * 128], in_=P[pslot][:, 0:128], func=AF.Identity, bias=ones[:, 0:1], scale=1.0),
                             reads=[f'P{pslot}', 's3_ones'], writes=['s3_bc'])
                    else:
                        S.op('act', lambda e, pslot=pslot, j=j, vi=vi, c8=c8: e.copy(out=bc[:, j, vi, c8 * 128:(c8 + 1) * 128], in_=P[pslot][:, 0:128]),
                             reads=[f'P{pslot}'], writes=['s3_bc'])
        yt = [sb2(f"s3_yt{i}", [128, 8, 128]) for i in range(2)]
        cand = [sb2(f"s3_cand{i}", [128, 4, 128]) for i in range(3)]
        selm = sb2("s3_selm", [128, 4])
        if 'selm' in dr:
            S.dma(lambda e: e.dma_start(out=selm[:], in_=dr['selm'][:, :]), writes=['s3_selm'])
        ci_ = 0
        xt = [sb2(f"s3_xt{i}", [128, 1024]) for i in range(2)]
        zt = [sb2(f"s3_zt{i}", [128, 1024]) for i in range(2)]
        x1t = [sb2(f"s3_x1{i}", [128, 1024]) for i in range(2)]
        h2t = [sb2(f"s3_h2{i}", [128, 1024]) for i in range(2)]
        hTf = [sb2(f"s3_hTf{i}", [128, 8, 128]) for i in range(2)]
        hTb = [sb2(f"s3_hTb{i}", [128, 8, 128], BF16) for i in range(2)]
        st6 = [sb2(f"s3_st{i}", [128, 2, 6]) for i in range(2)]
        mv = [sb2(f"s3_mv{i}", [128, 2]) for i in range(2)]
        rt = [sb2(f"s3_rt{i}", [128, 96]) for i in range(2)]
        for ti, (r0, n) in enumerate(S3_TILES):
            a = ti % 2
            j = 1 if ti == 0 else 0
            for r in range(4):
                for hf in range(2):
                    if 'yTs' in dr:
                        S.dma(lambda e, a=a, r=r, hf=hf, r0=r0, n=n: e.dma_start(out=yt[a][:, r * 2 + hf, 0:n], in_=dr['yTs'][r, hf * 128:(hf + 1) * 128, r0:r0 + n]), writes=[f's3_yt{a}'])
                        continue
                    cb = cand[ci_ % 3]; ck = f's3_cand{ci_%3}'; ci_ += 1
                    S.dma(lambda e, cb=cb, r=r, hf=hf, r0=r0, n=n: e.dma_start(out=cb[:, :, 0:n], in_=yG[r, :, hf * 128:(hf + 1) * 128, r0:r0 + n].rearrange("k p t -> p k t")), writes=[ck])
                    S.op('dve', lambda e, a=a, cb=cb, r=r, hf=hf, n=n: e.tensor_scalar(out=yt[a][:, r * 2 + hf, 0:n], in0=cb[:, 0, 0:n], scalar1=selm[:, 0:1], scalar2=None, op0=ALU.mult),
                         reads=[ck, 's3_selm'], writes=[f's3_yt{a}'])
                    for k in range(1, 4):
                        S.op('dve', lambda e, a=a, cb=cb, r=r, hf=hf, n=n, k=k: e.scalar_tensor_tensor(out=yt[a][:, r * 2 + hf, 0:n], in0=cb[:, k, 0:n], scalar=selm[:, k:k + 1], in1=yt[a][:, r * 2 + hf, 0:n], op0=ALU.mult, op1=ALU.add),
                             reads=[ck, 's3_selm', f's3_yt{a}'], writes=[f's3_yt{a}'])
            S.dma(lambda e, a=a, r0=r0, n=n: e.dma_start(out=xt[a][0:n, :], in_=x_sh[r0:r0 + n, :]), writes=[f's3_xt{a}'])
            for nch in range(2):
                for kc in range(8):
                    S.op('pe', lambda e, a=a, nch=nch, kc=kc, n=n: e.matmul(P[2 + nch][0:n, :], lhsT=yt[a][:, kc, 0:n], rhs=Wo[:, kc, nch * 512:(nch + 1) * 512], start=(kc == 0), stop=(kc == 7)),
                         reads=[f's3_yt{a}', 's3_Wo'], writes=[f'P{2+nch}'])
                S.op('dve', lambda e, a=a, nch=nch, n=n, j=j: e.tensor_tensor(out=zt[a][0:n, nch * 512:(nch + 1) * 512], in0=P[2 + nch][0:n, :], in1=bc[0:n, j, 0, nch * 512:(nch + 1) * 512], op=ALU.mult),
                     reads=[f'P{2+nch}', 's3_bc'], writes=[f's3_zt{a}'])
            if 'dbgA' in dr and ti == 1:
                S.dma(lambda e, a=a: e.dma_start(out=dr['dbgA'][0], in_=zt[a][:, :]), reads=[f's3_zt{a}'])
                S.dma(lambda e, a=a: e.dma_start(out=dr['dbgA'][1], in_=yt[a][:, :, :].rearrange("p k t -> p (k t)")), reads=[f's3_yt{a}'])
                S.dma(lambda e, a=a: e.dma_start(out=dr['dbgA'][2], in_=bc[:, 0, 0, :]), reads=['s3_bc'])
                S.dma(lambda e, a=a: e.dma_start(out=dr['dbgA'][3], in_=xt[a][:, :]), reads=[f's3_xt{a}'])
            S.op('dve', lambda e, a=a, n=n: e.scalar_tensor_tensor(out=zt[a][0:n, :], in0=xt[a][0:n, :], scalar=ALPHA, in1=zt[a][0:n, :], op0=ALU.mult, op1=ALU.add),
                 reads=[f's3_xt{a}', f's3_zt{a}'], writes=[f's3_zt{a}'])
            if 'dbgA' in dr and ti == 1:
                S.dma(lambda e, a=a: e.dma_start(out=dr['dbgA'][4], in_=zt[a][:, :]), reads=[f's3_zt{a}'])
            layer_norm_tile(S, zt[a], f's3_zt{a}', n, st6[a], mv[a], ln[:, 0, :], ln[:, 1, :], 's3_ln', x1t[a], f's3_x1{a}', f's3_a{a}')
            S.dma(lambda e, a=a, r0=r0, n=n: e.dma_start(out=x1_d[r0:r0 + n, :], in_=x1t[a][0:n, :]), reads=[f's3_x1{a}'])
            S.op('pool', lambda e, a=a, n=n, j=j: e.tensor_tensor(out=h2t[a][0:n, :], in0=x1t[a][0:n, :], in1=bc[0:n, j, 1, :], op=ALU.mult), reads=[f's3_x1{a}', 's3_bc'], writes=[f's3_h2{a}'])
            S.op('pool', lambda e, a=a, n=n, j=j: e.tensor_tensor(out=h2t[a][0:n, :], in0=h2t[a][0:n, :], in1=bc[0:n, j, 2, :], op=ALU.add), reads=[f's3_h2{a}', 's3_bc'], writes=[f's3_h2{a}'])
            for hf in range(2):
                for k4 in range(4):
                    kc = hf * 4 + k4
                    S.op('pe', lambda e, a=a, hf=hf, k4=k4, kc=kc, n=n: e.transpose(P[4 + hf][:, k4 * 128:k4 * 128 + n], h2t[a][0:n, kc * 128:(kc + 1) * 128], idj[0:n, 0, 0:n]),
                         reads=[f's3_h2{a}', 's3_idj'], writes=[f'P{4+hf}'])
                S.op('act', lambda e, a=a, hf=hf, n=n: e.copy(out=hTf[a][:, hf * 4:(hf + 1) * 4, 0:n], in_=P[4 + hf][:, :].rearrange("p (k t) -> p k t", t=128)[:, :, 0:n]),
                     reads=[f'P{4+hf}'], writes=[f's3_hTf{a}'])
            S.op('pool', lambda e, a=a, n=n: e.tensor_copy(out=hTb[a][:, :, 0:n], in_=hTf[a][:, :, 0:n]), reads=[f's3_hTf{a}'], writes=[f's3_hTb{a}'])
            S.dma(lambda e, a=a, r0=r0, n=n: e.dma_start(out=H2T_d[:, :, r0:r0 + n], in_=hTb[a][:, :, 0:n]), reads=[f's3_hTb{a}'])
            for kc in range(8):
                S.op('pe', lambda e, a=a, kc=kc, n=n: e.matmul(P[6 + a][0:n, 0:36], lhsT=hTf[a][:, kc, 0:n], rhs=Wr[:, kc, :], start=(kc == 0), stop=(kc == 7)),
                     reads=[f's3_hTf{a}', 's3_Wr'], writes=[f'P{6+a}'])
            R_ = rt[a]; rk = f's3_rt{a}'
            lg, gm, g1h, pen, m8, wv = R_[:, 0:36], R_[:, 36:40], R_[:, 40:44], R_[:, 44:48], R_[:, 48:56], R_[:, 56:64]
            msk = R_[:, 64:96]
            def rop(fn, eng='dve'):
                S.op(eng, fn, reads=[rk, f'P{6+a}', 's3_rb'], writes=[rk], hz=True)
            rop(lambda e, n=n, a=a, lg=lg: e.tensor_tensor(out=lg[0:n, :], in0=P[6 + a][0:n, 0:36], in1=rb[0:n, :], op=ALU.add))
            rop(lambda e, n=n, lg=lg, gm=gm: e.tensor_reduce(out=gm[0:n, 0:1], in_=lg[0:n, 0:4], axis=mybir.AxisListType.X, op=ALU.max))
            rop(lambda e, n=n, lg=lg, gm=gm, g1h=g1h: e.tensor_scalar(out=g1h[0:n, :], in0=lg[0:n, 0:4], scalar1=gm[0:n, 0:1], scalar2=None, op0=ALU.is_equal))
            rop(lambda e, n=n, gm=gm: e.tensor_scalar(out=gm[0:n, 1:2], in0=gm[0:n, 0:1], scalar1=-1.0, scalar2=None, op0=ALU.mult))
            rop(lambda e, n=n, lg=lg, gm=gm, pen=pen: e.activation(out=pen[0:n, :], in_=lg[0:n, 0:4], func=AF.Exp, bias=gm[0:n, 1:2], scale=1.0), eng='act')
            rop(lambda e, n=n, gm=gm, pen=pen: e.tensor_reduce(out=gm[0:n, 2:3], in_=pen[0:n, :], axis=mybir.AxisListType.X, op=ALU.add))
            rop(lambda e, n=n, gm=gm: e.reciprocal(out=gm[0:n, 2:3], in_=gm[0:n, 2:3]))
            rop(lambda e, n=n, g1h=g1h, pen=pen: e.tensor_scalar(out=pen[0:n, :], in0=g1h[0:n, :], scalar1=1e4, scalar2=-1e4, op0=ALU.mult, op1=ALU.add))
            rop(lambda e, n=n, lg=lg, pen=pen, msk=msk: e.tensor_tensor(out=msk[0:n, :].rearrange("p (g k) -> p g k", k=8), in0=lg[0:n, 4:36].rearrange("p (g k) -> p g k", k=8),
                                                                     in1=pen[0:n, :].unsqueeze(2).broadcast_to([n, 4, 8]), op=ALU.add))
            rop(lambda e, n=n, msk=msk, m8=m8: e.max(out=m8[0:n, :], in_=msk[0:n, :]))
            rop(lambda e, n=n, m8=m8, wv=wv: e.tensor_tensor(out=wv[0:n, 0:1], in0=m8[0:n, 1:2], in1=m8[0:n, 0:1], op=ALU.subtract))
            rop(lambda e, n=n, wv=wv: e.activation(out=wv[0:n, 0:1], in_=wv[0:n, 0:1], func=AF.Exp), eng='act')
            rop(lambda e, n=n, wv=wv: e.tensor_scalar(out=wv[0:n, 0:1], in0=wv[0:n, 0:1], scalar1=1.0, scalar2=None, op0=ALU.add))
            rop(lambda e, n=n, wv=wv: e.reciprocal(out=wv[0:n, 1:2], in_=wv[0:n, 0:1]))
            rop(lambda e, n=n, wv=wv: e.tensor_scalar(out=wv[0:n, 2:3], in0=wv[0:n, 1:2], scalar1=-1.0, scalar2=1.0, op0=ALU.mult, op1=ALU.add))
            rop(lambda e, n=n, wv=wv, gm=gm: e.tensor_scalar(out=wv[0:n, 1:3], in0=wv[0:n, 1:3], scalar1=gm[0:n, 2:3], scalar2=None, op0=ALU.mult))
            rop(lambda e, n=n, msk=msk, m8=m8, wv=wv, lg=lg: e.tensor_scalar(out=lg[0:n, 4:36], in0=msk[0:n, :], scalar1=m8[0:n, 0:1], scalar2=wv[0:n, 1:2], op0=ALU.is_equal, op1=ALU.mult))
            rop(lambda e, n=n, msk=msk, m8=m8, wv=wv: e.tensor_scalar(out=msk[0:n, :], in0=msk[0:n, :], scalar1=m8[0:n, 1:2], scalar2=wv[0:n, 2:3], op0=ALU.is_equal, op1=ALU.mult))
            S.op('dve', lambda e, n=n, ti=ti, lg=lg, msk=msk: e.tensor_tensor(out=WT[0:n, ti, :], in0=lg[0:n, 4:36], in1=msk[0:n, :], op=ALU.add), reads=[rk], writes=['s3_WT'], hz=True)

    if 'dbgWT' in dr:
        S.dma(lambda e: e.dma_start(out=dr['dbgWT'].rearrange("p (t k) -> p t k", k=32), in_=WT[:]), reads=['s3_WT'])
        S.barrier()
    if 'dbgA' in dr and 'wg' not in dr:
        return
    with Scope_(nc, S) as sb3:
        NG = 11
        acc = sb3("s3_acc", [128, NG, 1024])
        H2g = sb3("s3_H2g", [128, 8, 1408], BF16)
        Wb = [sb3(f"s3_Wb{i}", [128, 3, 4096], BF16) for i in range(2)]
        stg = [sb3(f"s3_stg{i}", [128, 4096]) for i in range(2)]
        HID = [sb3(f"s3_HID{i}", [128, 4, 512], BF16) for i in range(2)]
        sgt = [sb3(f"s3_sg{i}", [128, 512]) for i in range(2)]
        bc2 = sb3("s3_bc2", [128, 2, 1024])
        ln2 = sb3("s3_ln2", [128, 2, 1024])
        mTb = sb3("s3_mT2", [128, 48, 2])
        Dgb = [sb3(f"s3_D2{i}", [128, 128]) for i in range(2)]
        x1tb = [sb3(f"s3_x1b{i}", [128, 1024]) for i in range(2)]
        ot = [sb3(f"s3_ot{i}", [128, 1024]) for i in range(2)]
        st6b = [sb3(f"s3_st2{i}", [128, 2, 6]) for i in range(2)]
        mvb = [sb3(f"s3_mv2{i}", [128, 2]) for i in range(2)]
        S.dma(lambda e: e.dma_start(out=mTb[:], in_=modT_d.rearrange("p (c j) -> p c j", j=2)), writes=['s3_mT2'])
        S.dma(lambda e: e.dma_start(out=ln2[:], in_=dr['ln_bc'][2:4].rearrange("m p n -> p m n")), writes=['s3_ln2'])
        di = 0
        for j in range(2):
            for c8 in range(8):
                col = 5 * 8 + c8
                Dt = Dgb[di % 2]; dk = f's3_D2{di%2}'; pslot = di % 2; di += 1
                S.op('dve', lambda e, Dt=Dt, col=col, j=j: e.tensor_scalar(out=Dt[:], in0=idj[:, 0, :], scalar1=mTb[:, col, j:j + 1], scalar2=None, op0=ALU.mult),
                     reads=['s3_idj', 's3_mT2'], writes=[dk])
                S.op('pe', lambda e, Dt=Dt, pslot=pslot: e.matmul(P[pslot][:, 0:128], lhsT=ones[:], rhs=Dt[:], start=True, stop=True), reads=[dk, 's3_ones'], writes=[f'P{pslot}'])
                S.op('act', lambda e, pslot=pslot, j=j, c8=c8: e.copy(out=bc2[:, j, c8 * 128:(c8 + 1) * 128], in_=P[pslot][:, 0:128]), reads=[f'P{pslot}'], writes=['s3_bc2'])
        wcount = 0
        si = 0
        po = 0
        for g, tiles in enumerate(S3_GROUPS):
            c_lo = S3_TILES[tiles[0]][0]
            c_hi = S3_TILES[tiles[-1]][0] + S3_TILES[tiles[-1]][1]
            gw = c_hi - c_lo
            S.dma(lambda e, c_lo=c_lo, gw=gw: e.dma_start(out=H2g[:, :, 0:gw], in_=H2T_d[:, :, c_lo:c_lo + gw]), writes=['s3_H2g'])
            S.op('pool', lambda e: e.memset(acc[:], 0.0), writes=['s3_acc'])
            blocks = [tiles[i:i + 4] for i in range(0, len(tiles), 4)]
            for ex in range(32):
                wb = Wb[wcount % 2]; wk = f's3_Wb{wcount%2}'; wcount += 1
                for mi, (src, pat) in enumerate(((dr['wg'][ex], "(kc p) n -> p kc n"), (dr['wu'][ex], "(kc p) n -> p kc n"), (dr['wd'][ex], "(kc p) n -> p kc n"))):
                    sg_ = stg[si % 2]; sk = f's3_stg{si%2}'; si += 1
                    nk = 8 if mi < 2 else 4
                    S.dma(lambda e, sg_=sg_, src=src, pat=pat, nk=nk: e.dma_start(out=sg_[:, :].rearrange("p (k n) -> p k n", k=nk), in_=src.rearrange(pat, p=128)), writes=[sk])
                    S.op('pool', lambda e, sg_=sg_, wb=wb, mi=mi: e.tensor_copy(out=wb[:, mi, :], in_=sg_[:, :]), reads=[sk], writes=[wk])
                Wg = wb[:, 0, :].rearrange("p (k n) -> p k n", k=8)
                Wu = wb[:, 1, :].rearrange("p (k n) -> p k n", k=8)
                Wd = wb[:, 2, :].rearrange("p (k n) -> p k n", k=4)
                for blk in blocks:
                    b0 = S3_TILES[blk[0]][0] - c_lo
                    ncols = sum(S3_TILES[t][1] for t in blk)
                    hb = HID[po % 2]; hk = f's3_HID{po%2}'
                    for dc in range(4):
                        pg, pu = P[(dc % 2) * 2], P[(dc % 2) * 2 + 1]
                        for kc in range(8):
                            S.op('pe', lambda e, pg=pg, Wg=Wg, kc=kc, dc=dc, b0=b0, ncols=ncols: e.matmul(pg[:, 0:ncols], lhsT=Wg[:, kc, dc * 128:(dc + 1) * 128], rhs=H2g[:, kc, b0:b0 + ncols], start=(kc == 0), stop=(kc == 7)),
                                 reads=[wk, 's3_H2g'], writes=[f'P{(dc%2)*2}'])
                        for kc in range(8):
                            S.op('pe', lambda e, pu=pu, Wu=Wu, kc=kc, dc=dc, b0=b0, ncols=ncols: e.matmul(pu[:, 0:ncols], lhsT=Wu[:, kc, dc * 128:(dc + 1) * 128], rhs=H2g[:, kc, b0:b0 + ncols], start=(kc == 0), stop=(kc == 7)),
                                 reads=[wk, 's3_H2g'], writes=[f'P{(dc%2)*2+1}'])
                        sgb = sgt[dc % 2]; sgk = f's3_sg{dc%2}'
                        S.op('act', lambda e, sgb=sgb, pg=pg, ncols=ncols: e.activation(out=sgb[:, 0:ncols], in_=pg[:, 0:ncols], func=AF.Silu), reads=[f'P{(dc%2)*2}'], writes=[sgk])
                        S.op('dve', lambda e, hb=hb, dc=dc, pu=pu, sgb=sgb, ncols=ncols: e.tensor_tensor(out=hb[:, dc, 0:ncols], in0=pu[:, 0:ncols], in1=sgb[:, 0:ncols], op=ALU.mult),
                             reads=[f'P{(dc%2)*2+1}', sgk], writes=[hk])
                    off = 0
                    for t in blk:
                        n = S3_TILES[t][1]
                        tl = t - tiles[0]
                        for nch in range(2):
                            ps_ = 4 + po % 4; po_key = f'P{ps_}'
                            S.op('pe', lambda e: None, reads=[], writes=[]) if False else None
                            for dc in range(4):
                                S.op('pe', lambda e, ps_=ps_, hb=hb, dc=dc, off=off, n=n, Wd=Wd, nch=nch: e.matmul(P[ps_][0:n, :], lhsT=hb[:, dc, off:off + n], rhs=Wd[:, dc, nch * 512:(nch + 1) * 512], start=(dc == 0), stop=(dc == 3)),
                                     reads=[hk, wk], writes=[po_key])
                            S.op('dve', lambda e, ps_=ps_, n=n, tl=tl, nch=nch, t=t, ex=ex: e.scalar_tensor_tensor(out=acc[0:n, tl, nch * 512:(nch + 1) * 512], in0=P[ps_][0:n, :], scalar=WT[0:n, t, ex:ex + 1],
                                                                                                                in1=acc[0:n, tl, nch * 512:(nch + 1) * 512], op0=ALU.mult, op1=ALU.add),
                                 reads=[po_key, 's3_WT', 's3_acc'], writes=['s3_acc'])
                            po += 1
                        off += n
                    po += 1
            for t in tiles:
                r0, n = S3_TILES[t]
                a = t % 2
                tl = t - tiles[0]
                j = 1 if t == 0 else 0
                S.dma(lambda e, a=a, r0=r0, n=n: e.dma_start(out=x1tb[a][0:n, :], in_=x1_d[r0:r0 + n, :]), writes=[f's3_x1b{a}'])
                S.op('dve', lambda e, n=n, tl=tl, j=j: e.tensor_tensor(out=acc[0:n, tl, :], in0=acc[0:n, tl, :], in1=bc2[0:n, j, :], op=ALU.mult), reads=['s3_acc', 's3_bc2'], writes=['s3_acc'])
                S.op('dve', lambda e, a=a, n=n, tl=tl: e.scalar_tensor_tensor(out=acc[0:n, tl, :], in0=x1tb[a][0:n, :], scalar=ALPHA, in1=acc[0:n, tl, :], op0=ALU.mult, op1=ALU.add),
                     reads=['s3_acc', f's3_x1b{a}'], writes=['s3_acc'])
                layer_norm_tile(S, acc[:, tl, :], 's3_acc', n, st6b[a], mvb[a], ln2[:, 0, :], ln2[:, 1, :], 's3_ln2', ot[a], f's3_ot{a}', f's3_b{a}')
                S.dma(lambda e, a=a, r0=r0, n=n: e.dma_start(out=x_out[r0:r0 + n, :], in_=ot[a][0:n, :]), reads=[f's3_ot{a}'])


def body_A(nc, S, sb, ps):
    T = {}
    def din(name, shape, dt=F32):
        T[name] = nc.dram_tensor(name, list(shape), dt, kind="ExternalInput").ap()
    def dout(name, shape, dt=F32):
        T[name] = nc.dram_tensor(name, list(shape), dt, kind="ExternalOutput").ap()
    def dint(name, shape, dt=F32):
        T[name] = nc.dram_tensor(name, list(shape), dt, kind="Internal").ap()
    din('x_full', [TT, D]); din('cc', [128, 16]); din('w_mod', [D, 6 * D]); din('b_modT', [128, 48])
    din('w_c', [D, NCOL]); din('w_n', [D, 256]); din('ident2', [2, 128, 128]); din('na_bias', [2, 128, NCFG, 128])
    din('rw_par', [128, RW_NPAR]); din('rw_mats', [3, 64, 128]); din('rw_cst', [3, 128, 128])
    din('ml_par', [128, 12]); din('ml_cst', [3, 128, 128]); din('ml_nw', [128, 64]); din('rope', [2, 2, 64, 16384])
    dout('yT', [4, 256, NSH]); dout('modT_out', [128, 96])
    dint('pT', [2, NCOL, TT]); dint('pN', [2, TT, 256]); dint('rwF', [5, 2, TT, 64]); dint('rwBG', [2, 64, TT]); dint('mlQK', [2, 128, TT])
    P = [ps(f"P{i}", [128, 512]) for i in range(8)]
    dr = T
    with Scope(nc, S) as s1:
        stage1(nc, S, s1, P, dr)
    S.new_epoch()
    with Scope(nc, S) as s2:
        stage_na(nc, S, s2, P, dr)
    S.new_epoch()
    with Scope(nc, S) as s3:
        stage_rw(nc, S, s3, P, dr, Scope)
    S.new_epoch()
    with Scope(nc, S) as s4:
        stage_ml(nc, S, s4, P, dr, Scope)


def body_C(nc, S, sb, ps):
    T = {}
    def din(name, shape, dt=F32):
        T[name] = nc.dram_tensor(name, list(shape), dt, kind="ExternalInput").ap()
    def dout(name, shape, dt=F32):
        T[name] = nc.dram_tensor(name, list(shape), dt, kind="ExternalOutput").ap()
    def dint(name, shape, dt=F32):
        T[name] = nc.dram_tensor(name, list(shape), dt, kind="Internal").ap()
    din('yTs', [4, 256, NSH]); din('x_sh', [NSH, D]); din('modT_out', [128, 96]); din('ident2', [2, 128, 128])
    din('w_out_p', [D, D]); din('w_r', [D, 36]); din('r_b', [128, 36]); din('ln_bc', [4, 128, D])
    din('wg', [32, D, 512]); din('wu', [32, D, 512]); din('wd', [32, 512, D])
    dint('x1_scr', [NSH, D]); dint('H2T_scr', [128, 8, NSH], BF16)
    dout('x_out', [NSH, D])
    P = [ps(f"P{i}", [128, 512]) for i in range(8)]
    with Scope(nc, S) as s1:
        stage3(nc, S, s1, P, T, Scope, False)


_NC_CACHE = {}

def kernel(**inputs):
    inp = {k: np.asarray(v) for k, v in inputs.items()}
    if 'A' not in _NC_CACHE:
        _NC_CACHE['A'] = build_program(body_A)
        _NC_CACHE['C'] = build_program(body_C)
    ncA, ncC = _NC_CACHE['A'], _NC_CACHE['C']
    perm = w_out_perm_rows()
    rope = rope_tables(); id2 = ident2(); rwc, mlc = rw_consts(), ml_consts()
    f32 = lambda a: np.ascontiguousarray(a, dtype=np.float32)
    x_cur = inp['x']; ctx_cur = inp['ctx']
    for l in range(2):
        mapsA = []
        for i in range(8):
            b, j = i // 4, i % 4
            cols_c, cols_n = colmap(j)
            rwp, rwm = rw_params(inp, l, j); mlp, mlnw = ml_params(inp, l, j)
            mapsA.append(dict(
                x_full=x_full_of(x_cur[b], ctx_cur[b]),
                cc=f32(np.concatenate([inp['c'][b].reshape(8, 128).T, inp['c_ctx'].reshape(8, 128).T], 1)),
                w_mod=f32(inp['w_mod'][l]), b_modT=f32(inp['b_mod'][l].reshape(48, 128).T),
                w_c=f32(inp['w_in'][l][:, cols_c]), w_n=f32(inp['w_in'][l][:, cols_n]), ident2=id2,
                na_bias=na_bias_tables(inp['na_rpb'][l][2 * j:2 * j + 2]),
                rw_par=rwp, rw_mats=rwm, rw_cst=rwc, ml_par=mlp, ml_cst=mlc, ml_nw=mlnw, rope=rope))
        resA = run_bass_kernel_spmd(ncA, mapsA, core_ids=list(range(8)))
        mapsC = []
        w_r = f32(np.concatenate([inp['router_group_w'][l], inp['router_expert_w'][l]], 1))
        r_b = f32(np.repeat(np.concatenate([inp['router_group_b'][l], inp['router_expert_b'][l]])[None], 128, 0))
        ln_bc = f32(np.stack([np.repeat(inp[k][l][None], 128, 0) for k in ('ln1_g', 'ln1_b', 'ln2_g', 'ln2_b')]))
        wop = f32(inp['w_out'][l][perm])
        for i in range(8):
            b, j = i // 4, i % 4
            yTs = f32(np.stack([resA.results[4 * b + r]['yT'][j] for r in range(4)]))
            x_sh = f32(np.concatenate([ctx_cur[b, 64 * j:64 * j + 64], x_cur[b, 4096 * j:4096 * (j + 1)]], 0))
            mapsC.append(dict(yTs=yTs, x_sh=x_sh, modT_out=resA.results[i]['modT_out'], ident2=id2, w_out_p=wop, w_r=w_r, r_b=r_b, ln_bc=ln_bc,
                              wg=f32(inp['exp_w_gate'][l]), wu=f32(inp['exp_w_up'][l]), wd=f32(inp['exp_w_down'][l])))
        resC = run_bass_kernel_spmd(ncC, mapsC, core_ids=list(range(8)))
        x_new = np.empty_like(x_cur); c_new = np.empty_like(ctx_cur)
        for i in range(8):
            b, j = i // 4, i % 4
            xo = resC.results[i]['x_out']
            c_new[b, 64 * j:64 * j + 64] = xo[:64]
            x_new[b, 4096 * j:4096 * (j + 1)] = xo[64:]
        x_cur, ctx_cur = x_new, c_new
    return np.ascontiguousarray(x_cur, dtype=np.float32)
```
